# Optimizing a Trainium2 kernel written in Bass

```python
import jax
import jax.numpy as jnp
from jax import lax
import numpy as np

D_MODEL = 1024
BATCH = 16
SEQ = 2048
DEPTH = 2

HEAD_DIM = 64
SB_HEADS = 8
FOX_HEADS = 8
DSA_HEADS = 8
DSA_KV_HEADS = 2
DSA_GROUP = DSA_HEADS // DSA_KV_HEADS
IDX_HEADS = 4
IDX_DIM = 64
BRANCH_WIDTH = SB_HEADS * HEAD_DIM
N_BRANCH = 3
D_FF = 4 * D_MODEL
ROPE_THETA = 500000.0
ROT_DIM = HEAD_DIM // 4
Q_BLOCK = 128
TOPK_MAX = 256
NORM_EPS = 1e-6

COL_SIZES = (
    SB_HEADS * HEAD_DIM, SB_HEADS * HEAD_DIM, SB_HEADS * HEAD_DIM,
    FOX_HEADS * HEAD_DIM, FOX_HEADS * HEAD_DIM, FOX_HEADS * HEAD_DIM,
    FOX_HEADS,
    DSA_HEADS * HEAD_DIM, DSA_KV_HEADS * HEAD_DIM, DSA_KV_HEADS * HEAD_DIM,
    IDX_HEADS * IDX_DIM, IDX_DIM, IDX_HEADS,
    N_BRANCH * D_MODEL,
)
COL_TOTAL = sum(COL_SIZES)
COL_SPLITS = tuple(sum(COL_SIZES[:i + 1]) for i in range(len(COL_SIZES) - 1))

kernel_name = 'hybrid_gated_sb_fox_dsa_trunk'


def _rmsnorm(x, g):
    xf = x.astype(jnp.float32)
    y = xf * lax.rsqrt(jnp.mean(xf * xf, axis=-1, keepdims=True) + NORM_EPS)
    return (y * g.astype(jnp.float32)).astype(x.dtype)


def _partial_rope(x, positions):
    half = ROT_DIM // 2
    inv_freq = ROPE_THETA ** (-jnp.arange(0, ROT_DIM, 2, dtype=jnp.float32) / ROT_DIM)
    ang = positions.astype(jnp.float32)[:, :, None] * inv_freq
    cos = jnp.cos(ang)[:, :, None, :]
    sin = jnp.sin(ang)[:, :, None, :]
    xf = x.astype(jnp.float32)
    x1, x2, rest = xf[..., :half], xf[..., half:ROT_DIM], xf[..., ROT_DIM:]
    out = jnp.concatenate([x1 * cos - x2 * sin, x2 * cos + x1 * sin, rest], axis=-1)
    return out.astype(x.dtype)


def _to_blocks(a):
    b, s = a.shape[0], a.shape[1]
    return jnp.moveaxis(a.reshape(b, s // Q_BLOCK, Q_BLOCK, *a.shape[2:]), 1, 0)


def _from_blocks(a):
    a = jnp.moveaxis(a, 0, 1)
    return a.reshape(a.shape[0], a.shape[1] * a.shape[2], *a.shape[3:])


def _stick_breaking_attention(q, k, v):
    b, s, h, dh = q.shape
    scale = dh ** -0.5
    kf = k.astype(jnp.float32)
    vf = v.astype(jnp.float32)
    s_idx = jnp.arange(s)
    t_blk = jnp.arange(s).reshape(s // Q_BLOCK, Q_BLOCK)

    def block(args):
        qb, tb = args
        z = jnp.einsum('bthd,bshd->bhts', qb.astype(jnp.float32), kf) * scale
        strict = s_idx[None, :] < tb[:, None]
        log_beta = jax.nn.log_sigmoid(z)
        log_1m = jnp.where(strict, jax.nn.log_sigmoid(-z), 0.0)
        later = lax.cumsum(log_1m, axis=3, reverse=True) - log_1m
        w = jnp.where(strict, jnp.exp(log_beta + later), 0.0)
        return jnp.einsum('bhts,bshd->bthd', w, vf)

    out = lax.map(block, (_to_blocks(q), t_blk))
    return _from_blocks(out).reshape(b, s, h * dh).astype(q.dtype)


def _forgetting_attention(q, k, v, log_f):
    b, s, h, dh = q.shape
    scale = dh ** -0.5
    kf = k.astype(jnp.float32)
    vf = v.astype(jnp.float32)
    c = jnp.cumsum(log_f, axis=1).transpose(0, 2, 1)
    c_blk = jnp.moveaxis(c.reshape(b, h, s // Q_BLOCK, Q_BLOCK), 2, 0)
    s_idx = jnp.arange(s)
    t_blk = jnp.arange(s).reshape(s // Q_BLOCK, Q_BLOCK)

    def block(args):
        qb, cb, tb = args
        z = jnp.einsum('bthd,bshd->bhts', qb.astype(jnp.float32), kf) * scale
        z = z + cb[..., None] - c[:, :, None, :]
        causal = s_idx[None, :] <= tb[:, None]
        p = jax.nn.softmax(jnp.where(causal, z, -jnp.inf), axis=-1)
        return jnp.einsum('bhts,bshd->bthd', p, vf)

    out = lax.map(block, (_to_blocks(q), c_blk, t_blk))
    return _from_blocks(out).reshape(b, s, h * dh).astype(q.dtype)


def _indexed_sparse_attention(q, k, v, q_idx, k_idx, w_idx, top_k):
    b, s, g, r, dh = q.shape
    scale = dh ** -0.5
    kif = k_idx.astype(jnp.float32)
    s_idx = jnp.arange(s)
    t_blk = jnp.arange(s).reshape(s // Q_BLOCK, Q_BLOCK)
    gather = jax.vmap(lambda a, i: a[i])

    def block(args):
        qb, qib, wb, tb = args
        dots = jnp.einsum('bthi,bsi->bths', qib.astype(jnp.float32), kif) * (IDX_DIM ** -0.5)
        score = jnp.einsum('bth,bths->bts', wb.astype(jnp.float32) * (IDX_HEADS ** -0.5),
                           jax.nn.relu(dots))
        causal = s_idx[None, :] <= tb[:, None]
        score = jnp.where(causal[None], score, -jnp.inf)
        _, sel = lax.top_k(score, top_k)
        valid = sel <= tb[None, :, None]
        k_sel = gather(k, sel).astype(jnp.float32)
        v_sel = gather(v, sel).astype(jnp.float32)
        z = jnp.einsum('btgrd,btkgd->btgrk', qb.astype(jnp.float32), k_sel) * scale
        z = jnp.where(valid[:, :, None, None, :], z, -jnp.inf)
        p = jax.nn.softmax(z, axis=-1)
        return jnp.einsum('btgrk,btkgd->btgrd', p, v_sel)

    out = lax.map(block, (_to_blocks(q), _to_blocks(q_idx), _to_blocks(w_idx), t_blk))
    return _from_blocks(out).reshape(b, s, g * r * dh).astype(q.dtype)


def _layer(x, positions, g_mix, w_in, b_forget, w_branch, w_out, g_mlp, w_up, w_down):
    b, s, _ = x.shape
    h = _rmsnorm(x, g_mix)
    proj = h @ w_in
    (qa, ka, va, qb, kb, vb, fl, qc, kc, vc, qi, ki, wi, gl) = jnp.split(proj, COL_SPLITS, axis=-1)

    def heads(a, n):
        return a.reshape(b, s, n, -1)

    br_a = _stick_breaking_attention(heads(qa, SB_HEADS), heads(ka, SB_HEADS), heads(va, SB_HEADS))
    log_f = jax.nn.log_sigmoid((fl + b_forget).astype(jnp.float32))
    br_b = _forgetting_attention(heads(qb, FOX_HEADS), heads(kb, FOX_HEADS),
                                 heads(vb, FOX_HEADS), log_f)
    qc = _partial_rope(heads(qc, DSA_HEADS), positions).reshape(b, s, DSA_KV_HEADS, DSA_GROUP, HEAD_DIM)
    kc = _partial_rope(heads(kc, DSA_KV_HEADS), positions)
    vc = heads(vc, DSA_KV_HEADS)
    qi = _partial_rope(heads(qi, IDX_HEADS), positions)
    ki = _partial_rope(ki[:, :, None, :], positions)[:, :, 0, :]
    top_k = min(TOPK_MAX, s // 4)
    br_c = _indexed_sparse_attention(qc, kc, vc, qi, ki, wi, top_k)

    gates = jax.nn.sigmoid(gl.reshape(b, s, N_BRANCH, D_MODEL))
    y = (gates[:, :, 0, :] * (br_a @ w_branch[0])
         + gates[:, :, 1, :] * (br_b @ w_branch[1])
         + gates[:, :, 2, :] * (br_c @ w_branch[2]))
    x = x + y @ w_out

    h2 = _rmsnorm(x, g_mlp)
    x = x + jnp.square(jax.nn.relu(h2 @ w_up)) @ w_down
    return x


def setup_inputs(seed: int = 0) -> dict:
    key = jax.random.key(seed)
    ks = jax.random.split(key, 12)
    f32 = jnp.float32
    x = jax.random.normal(ks[0], (BATCH, SEQ, D_MODEL), f32)
    positions = jnp.broadcast_to(jnp.arange(SEQ, dtype=jnp.int32), (BATCH, SEQ))
    g_mix = 1.0 + 0.02 * jax.random.normal(ks[1], (DEPTH, D_MODEL), f32)
    w_in = jax.random.normal(ks[2], (DEPTH, D_MODEL, COL_TOTAL), f32) * D_MODEL ** -0.5
    b_forget = jax.random.uniform(ks[3], (DEPTH, FOX_HEADS), f32, minval=1.0, maxval=4.0)
    w_branch = jax.random.normal(ks[4], (DEPTH, N_BRANCH, BRANCH_WIDTH, D_MODEL), f32) * BRANCH_WIDTH ** -0.5
    w_out = jax.random.normal(ks[5], (DEPTH, D_MODEL, D_MODEL), f32) * D_MODEL ** -0.5
    g_mlp = 1.0 + 0.02 * jax.random.normal(ks[6], (DEPTH, D_MODEL), f32)
    w_up = jax.random.normal(ks[7], (DEPTH, D_MODEL, D_FF), f32) * D_MODEL ** -0.5
    w_down = jax.random.normal(ks[8], (DEPTH, D_FF, D_MODEL), f32) * D_FF ** -0.5
    g_final = 1.0 + 0.02 * jax.random.normal(ks[9], (D_MODEL,), f32)
    return {'x': x, 'positions': positions, 'g_mix': g_mix, 'w_in': w_in,
            'b_forget': b_forget, 'w_branch': w_branch, 'w_out': w_out,
            'g_mlp': g_mlp, 'w_up': w_up, 'w_down': w_down, 'g_final': g_final}


def reference(x, positions, g_mix, w_in, b_forget, w_branch, w_out, g_mlp, w_up, w_down, g_final):
    for layer in range(DEPTH):
        x = _layer(x, positions, g_mix[layer], w_in[layer], b_forget[layer], w_branch[layer],
                   w_out[layer], g_mlp[layer], w_up[layer], w_down[layer])
    return _rmsnorm(x, g_final)
```

```python
import numpy as np
from contextlib import ExitStack
import concourse.bass as bass
import concourse.mybir as mybir
from concourse.bass_utils import run_bass_kernel_spmd

F32 = mybir.dt.float32
BF16 = mybir.dt.bfloat16
I32 = mybir.dt.int32
U32 = mybir.dt.uint32
AF = mybir.ActivationFunctionType
ALU = mybir.AluOpType
AX = mybir.AxisListType

CE = ('pe', 'act', 'dve', 'pool')
NCE = len(CE)
EPOCH = 4000
NDSEM = 12


class Buf:
    __slots__ = ('name', 'w', 'rs')

    def __init__(self, name=''):
        self.name = name
        self.w = None
        self.rs = []


class Op:
    __slots__ = ('eng', 'q', 'fn', 'idx', 'deps', 'waits', 'marked', 'semi', 'semv', 'clock', 'isdma', 'dnum')


class Sch:
    def __init__(self, nc):
        self.nc = nc
        self.ops = []
        self.cnt = {e: 0 for e in CE}
        self.last = {e: None for e in CE}
        self.ndma = {'sync': 0, 'pool': 0, 'act': 0}
        self.dmas = {'sync': [], 'pool': [], 'act': []}

    def _rec(self, o, reads, writes):
        deps = {}
        for b in reads:
            if b.w is not None:
                deps[id(b.w)] = b.w
        for b in writes:
            if b.w is not None:
                deps[id(b.w)] = b.w
            for r in b.rs:
                deps[id(r)] = r
        deps.pop(id(o), None)
        o.deps = list(deps.values())
        for b in reads:
            b.rs.append(o)
        for b in writes:
            b.w = o
            b.rs = []
        self.ops.append(o)

    def op(self, eng, fn, reads=(), writes=()):
        o = Op()
        o.eng = eng; o.q = eng; o.fn = fn; o.isdma = False
        self.cnt[eng] += 1
        o.idx = self.cnt[eng]
        o.marked = False
        self._rec(o, reads, writes)
        self.last[eng] = o
        return o

    def dma(self, fn, reads=(), writes=(), q='sync'):
        o = Op()
        o.eng = 'dma'; o.q = q; o.fn = fn; o.isdma = True
        o.dnum = self.ndma[q]
        self.ndma[q] += 1
        self.dmas[q].append(o)
        o.idx = 0
        o.marked = True
        self._rec(o, reads, writes)
        return o

    def fence(self):
        lst = [self.last[e] for e in CE if self.last[e] is not None]
        dl = []
        for q in self.dmas:
            dl += self.dmas[q][-NDSEM:]
        for e in CE + ('sync',):
            o = Op()
            o.eng = e; o.q = e; o.fn = None; o.isdma = False
            o.idx = 0
            o.marked = False
            o.deps = [d for d in lst if d.eng != e] + list(dl)
            self.ops.append(o)

    def plan(self):
        ei = {e: i for i, e in enumerate(CE)}
        run = {q: [0] * NCE for q in ('pe', 'act', 'dve', 'pool', 'sync')}
        seen_dma = {q: set() for q in run}
        for o in self.ops:
            q = o.q
            rc = run[q]
            waits = []
            for d in o.deps:
                if d.isdma:
                    if id(d) in seen_dma[q]:
                        continue
                    seen_dma[q].add(id(d))
                    waits.append(d)
                    dc = d.clock
                    for i in range(NCE):
                        if dc[i] > rc[i]:
                            rc[i] = dc[i]
                else:
                    if d.eng == 'pe' and o.eng == 'pe':
                        continue
                    j = ei[d.eng]
                    if rc[j] >= d.idx:
                        continue
                    waits.append(d)
                    d.marked = True
                    dc = d.clock
                    for i in range(NCE):
                        if dc[i] > rc[i]:
                            rc[i] = dc[i]
            best = {}
            fin = []
            for d in waits:
                if d.isdma:
                    fin.append(d)
                elif d.eng not in best or best[d.eng].idx < d.idx:
                    best[d.eng] = d
            fin.extend(best.values())
            o.waits = fin
            o.clock = list(rc)
            if not o.isdma and o.fn is not None:
                o.clock[ei[o.eng]] = o.idx

    def emit(self, stack):
        nc = self.nc
        self.plan()
        sems = {}

        def getsem(name):
            if name not in sems:
                sems[name] = stack.enter_context(nc.semaphore(name))
            return sems[name]

        mcount = {e: 0 for e in CE}
        for o in self.ops:
            if o.isdma:
                k = o.dnum % NDSEM
                o.semi = 'd_%s_%d' % (o.q, k)
                o.semv = 16 * (o.dnum // NDSEM + 1)
            elif o.marked:
                mcount[o.eng] += 1
                ep = (mcount[o.eng] - 1) // EPOCH
                o.semi = 'c_%s_%d' % (o.eng, ep)
                o.semv = mcount[o.eng] - ep * EPOCH
        for o in self.ops:
            if o.isdma or o.marked:
                getsem(o.semi)
        byq = {q: [] for q in ('pe', 'act', 'dve', 'pool', 'sync')}
        for o in self.ops:
            byq[o.q].append(o)
        self.nwaits = 0

        def run_queue(eng, lst):
            ring = {}
            for o in lst:
                for d in o.waits:
                    eng.wait_ge(sems[d.semi], d.semv)
                    self.nwaits += 1
                if o.isdma:
                    k = o.dnum % NDSEM
                    if k in ring:
                        p = ring[k]
                        eng.wait_ge(sems[p.semi], p.semv)
                    ring[k] = o
                    o.fn(eng).then_inc(sems[o.semi], 16)
                elif o.fn is not None:
                    ins = o.fn(eng)
                    if o.marked:
                        ins.then_inc(sems[o.semi], 1)

        with nc.Block() as block:
            @block.sync
            def _(e):
                run_queue(e, byq['sync'])
                for q in ('sync', 'pool', 'act'):
                    for o in self.dmas[q][-NDSEM:]:
                        e.wait_ge(sems[o.semi], o.semv)

            @block.tensor
            def _(e):
                run_queue(e, byq['pe'])

            @block.scalar
            def _(e):
                run_queue(e, byq['act'])

            @block.vector
            def _(e):
                run_queue(e, byq['dve'])

            @block.gpsimd
            def _(e):
                run_queue(e, byq['pool'])


SL = 2048
DM = 1024
NT = 16
NB = 4
KC = 8
QA, KA, VA, QB, KB, VB, FLC, CC, GATE = 0, 512, 1024, 1536, 2048, 2560, 3072, 3080, 4172
NCOL = 7244
CW = 1092
SCALE = 0.125
NEG = -1.0e30
NBIS = 22
TWO_PI = float(2 * np.pi)
PI = float(np.pi)


class Tl:
    def __init__(self, t, name=''):
        self.t = t
        self.b = Buf(name)

    def __getitem__(self, k):
        return self.t[k]


class StopBuild(Exception):
    pass


def build(NSEQ=2, DEPTH=2, dbg=False, stop=99, final=True):
    return _build(NSEQ, DEPTH, dbg, stop, final)


def _build(NSEQ, DEPTH, dbg, stop, final=True):
    nc = bass.Bass("TRN2", target_bir_lowering=False)

    def dram(name, shape, dtype, kind):
        return nc.dram_tensor(name, shape, dtype, kind=kind).ap()

    x_d = dram("x", [NSEQ, SL, DM], F32, "ExternalInput")
    pos_d = dram("positions", [NSEQ, SL], I32, "ExternalInput")
    gmix_d = dram("g_mix", [DEPTH, DM], F32, "ExternalInput")
    win_d = dram("w_in", [DEPTH, DM, NCOL], F32, "ExternalInput")
    bf_d = dram("b_forget", [DEPTH, 8], F32, "ExternalInput")
    wbr_d = dram("w_branch", [DEPTH, 3, 512, DM], F32, "ExternalInput")
    wout_d = dram("w_out", [DEPTH, DM, DM], F32, "ExternalInput")
    gmlp_d = dram("g_mlp", [DEPTH, DM], F32, "ExternalInput")
    wup_d = dram("w_up", [DEPTH, DM, 4 * DM], F32, "ExternalInput")
    wdn_d = dram("w_down", [DEPTH, 4 * DM, DM], F32, "ExternalInput")
    gfin_d = dram("g_final", [DM], F32, "ExternalInput")
    out_d = dram("out", [NSEQ, SL, DM], F32, "ExternalOutput")
    winb = dram("winb", [2, DM, NCOL], BF16, "Internal")
    wbrb = dram("wbrb", [2, 3, 512, DM], BF16, "Internal")
    woutb = dram("woutb", [2, DM, DM], BF16, "Internal")
    wupb = dram("wupb", [2, DM, 4 * DM], BF16, "Internal")
    wdnb = dram("wdnb", [2, 4 * DM, DM], BF16, "Internal")
    xs_d = dram("xs", [NSEQ, KC, 128, SL], F32, "Internal")
    aug_d = dram("augd", [3, 8, SL], BF16, "Internal")
    if dbg:
        dbg_h = dram("dbg_h", [128, KC, SL], F32, "ExternalOutput")
        dbg_br = dram("dbg_br", [3, 128, 4, SL], F32, "ExternalOutput")
        dbg_x = dram("dbg_x", [KC, 128, SL], F32, "ExternalOutput")

    top = ExitStack()
    S = Sch(nc)
    uid = [0]

    cur = [16384 + 256]
    peak = [0]
    LIMIT = 229376

    def sb(st, shape, dtype, name=None):
        uid[0] += 1
        nm = "%s_%d" % (name or 't', uid[0])
        isz = 2 if dtype == BF16 else 4
        nb = int(np.prod(shape[1:])) * isz
        nb = (nb + 63) // 64 * 64
        off = cur[0]
        cur[0] += nb
        peak[0] = max(peak[0], cur[0])
        assert cur[0] <= LIMIT, ("SBUF overflow", nm, cur[0])
        st.callback(lambda off=off: cur.__setitem__(0, off))
        return Tl(nc.alloc_sbuf_tensor_at(nm, shape, dtype, offset=off), nm)

    def bl(ts):
        return [t.b if isinstance(t, Tl) else t for t in ts]

    def mm(out, lhsT, rhs, start, stop, r, w):
        S.op('pe', lambda e: e.matmul(out, lhsT=lhsT, rhs=rhs, start=start, stop=stop), bl(r), bl(w))

    def tr(out, in_, ident, r, w):
        S.op('pe', lambda e: e.transpose(out=out, in_=in_, identity=ident), bl(r), bl(w))

    def act(out, in_, func, r, w, bias=None, scale=None):
        kw = {}
        if bias is not None:
            kw['bias'] = bias
        if scale is not None:
            kw['scale'] = scale
        S.op('act', lambda e: e.activation(out=out, in_=in_, func=func, **kw), bl(r), bl(w))

    def tt(eng, out, in0, in1, op, r, w):
        S.op(eng, lambda e: e.tensor_tensor(out=out, in0=in0, in1=in1, op=op), bl(r), bl(w))

    def ts(eng, out, in0, s1, s2, op0, op1, r, w, accum=None):
        kw = {}
        if op1 is not None:
            kw['op1'] = op1
        if accum is not None:
            kw['accum_out'] = accum
        S.op(eng, lambda e: e.tensor_scalar(out=out, in0=in0, scalar1=s1, scalar2=s2, op0=op0, **kw), bl(r), bl(w))

    def stt(eng, out, in0, scalar, in1, op0, op1, r, w):
        S.op(eng, lambda e: e.scalar_tensor_tensor(out=out, in0=in0, scalar=scalar, in1=in1, op0=op0, op1=op1), bl(r), bl(w))

    def cpy(eng, out, in_, r, w):
        if eng == 'act':
            S.op('act', lambda e: e.activation(out=out, in_=in_, func=AF.Copy), bl(r), bl(w))
        else:
            S.op(eng, lambda e: e.tensor_copy(out=out, in_=in_), bl(r), bl(w))

    def mset(eng, ap, val, w):
        S.op(eng, lambda e: e.memset(ap, val), [], bl(w))

    def asel(out, in_, pattern, cmp, fill, base, cm, r, w):
        S.op('pool', lambda e: e.affine_select(out=out, in_=in_, pattern=pattern, compare_op=cmp, fill=fill,
                                               base=base, channel_multiplier=cm), bl(r), bl(w))

    def dma(out, in_, r, w, q='sync'):
        S.dma(lambda e: e.dma_start(out=out, in_=in_), bl(r), bl(w), q=q)

    PS = [Tl(top.enter_context(nc.psum_tensor("ps%d" % i, [128, 512], F32)), "ps%d" % i) for i in range(8)]

    def psb(i):
        return PS[i].t[:].bitcast(BF16)

    identF = sb(top, [128, 128], F32, "identF")
    identB = sb(top, [128, 128], BF16, "identB")
    onesF = sb(top, [128, 128], F32, "onesF")
    onesB = sb(top, [128, 128], BF16, "onesB")
    triU = sb(top, [128, 128], BF16, "triU")
    opad = [sb(top, [128, 128], BF16, "opad%d" % i) for i in range(2)]
    ones3 = sb(top, [3, 128], BF16, "ones3")
    gst = sb(top, [40, 128], F32, "gst")
    gT = sb(top, [128, 40], F32, "gT")
    invf = sb(top, [128, 8], F32, "invf")
    bfneg = sb(top, [8, 2], F32, "bfneg")
    pow2 = sb(top, [128, NBIS + 1], F32, "pow2")
    tauc = sb(top, [128, 1], F32, "tauc")
    rden = sb(top, [128, 512], F32, "rden")
    BIG = 30000.0
    cmask = sb(top, [128, 128], F32, "cmask")
    mset('pool', cmask[:], 0.0, [cmask])
    asel(cmask[:], cmask[:], [[-1, 128]], ALU.is_ge, NEG, 0, 1, [cmask], [cmask])
    maskS = [sb(top, [128, 512], BF16, "maskS%d" % j) for j in range(4)]
    posA = [sb(top, [128, 512], BF16, "posA%d" % j) for j in range(4)]
    negB = [sb(top, [128, 512], BF16, "negB%d" % j) for j in range(4)]
    for j in range(4):
        mset('pool', maskS[j][:], 1.0, [maskS[j]])
        asel(maskS[j][:], maskS[j][:], [[1, 512]], ALU.is_gt, 0.0, -128 * j, -1, [maskS[j]], [maskS[j]])
        mset('pool', posA[j][:], 0.0, [posA[j]])
        asel(posA[j][:], posA[j][:], [[1, 512]], ALU.is_gt, BIG, -128 * j, -1, [posA[j]], [posA[j]])
        mset('pool', negB[j][:], 0.0, [negB[j]])
        asel(negB[j][:], negB[j][:], [[1, 512]], ALU.is_ge, -BIG, -128 * j, -1, [negB[j]], [negB[j]])

    for t_, dtv in ((identF, 0.0), (identB, 0.0)):
        mset('pool', t_[:], 0.0, [t_])
        asel(t_[:], t_[:], [[-1, 128]], ALU.not_equal, 1.0, 0, 1, [t_], [t_])
    mset('pool', onesF[:], 1.0, [onesF])
    mset('pool', onesB[:], 1.0, [onesB])
    mset('pool', ones3[:], 1.0, [ones3])
    mset('pool', triU[:], 1.0, [triU])
    asel(triU[:], triU[:], [[-1, 128]], ALU.is_gt, 0.0, 0, 1, [triU], [triU])
    for i in range(2):
        mset('pool', opad[i][:], 0.0, [opad[i]])
        mset('pool', opad[i][:, i * 64:(i + 1) * 64], 1.0, [opad[i]])
    invfreq = (np.float32(500000.0) ** (-(np.arange(0, 16, 2, dtype=np.float32)) / np.float32(16))).astype(np.float32)
    for j in range(8):
        mset('pool', invf[:, j:j + 1], float(invfreq[j]), [invf])
    for k in range(NBIS + 1):
        mset('pool', pow2[:, k:k + 1], float(2.0 ** -(k + 1)), [pow2])
    mset('pool', tauc[:], -1.0e29, [tauc])
    for r_, src in ((0, gmix_d[0]), (1, gmix_d[DEPTH - 1]), (2, gmlp_d[0]), (3, gmlp_d[DEPTH - 1]), (4, gfin_d)):
        dma(gst[r_ * 8:(r_ + 1) * 8, :], src.rearrange("(c p) -> c p", p=128), [], [gst])
    tr(PS[0][:, 0:40], gst[0:40, :], identF[0:40, 0:40], [gst, identF], [PS[0]])
    cpy('dve', gT[:], PS[0][:, 0:40], [PS[0]], [gT])
    for l in range(DEPTH):
        dma(bfneg[:, l:l + 1], bf_d[l].rearrange("(h o) -> h o", o=1), [], [bfneg])
    ts('dve', bfneg[:, 0:DEPTH], bfneg[:, 0:DEPTH], -1.0, None, ALU.mult, None, [bfneg], [bfneg])

    wbuf = {}

    def cast2d(dst, src, rows, key):
        b = Buf(key)
        wbuf[key] = b
        for r0 in range(0, rows, 128):
            dma(dst[r0:r0 + 128, :], src[r0:r0 + 128, :], [], [b], q='pool')

    for l in range(DEPTH):
        cast2d(winb[l], win_d[l], DM, ('in', l))
        cast2d(wbrb[l].rearrange("b k f -> (b k) f"), wbr_d[l].rearrange("b k f -> (b k) f"), 1536, ('br', l))
        cast2d(woutb[l], wout_d[l], DM, ('out', l))
        cast2d(wupb[l], wup_d[l], DM, ('up', l))
        cast2d(wdnb[l], wdn_d[l], 4 * DM, ('dn', l))

    def wload(tile, dst_ap, key, src2d, c0, c1):
        dma(dst_ap, src2d[:, c0:c1].rearrange("(kc p) f -> p kc f", p=128), [wbuf[key]], [tile])

    def norm_block(st_tmp, xblk, gidx, outs, out_bufs, ps_i, tmp):
        sq, rs, rs2 = tmp
        act(sq[:], xblk[:], AF.Square, [xblk], [sq])
        for c in range(KC):
            mm(PS[ps_i][:], onesF[:], sq[:, c, :], c == 0, c == KC - 1, [onesF, sq], [PS[ps_i]])
        ts('dve', rs[:], PS[ps_i][:], 1.0 / DM, 1e-6, ALU.mult, ALU.add, [PS[ps_i]], [rs])
        act(rs2[:], rs[:], AF.Sqrt, [rs], [rs2])
        S.op('dve', lambda e: e.reciprocal(out=rs[:], in_=rs2[:]), bl([rs2]), bl([rs]))
        for c in range(KC):
            stt('dve', outs(c), xblk[:, c, :], gT[:, gidx * 8 + c:gidx * 8 + c + 1], rs[:], ALU.mult, ALU.mult,
                [xblk, gT, rs], out_bufs)

    def run_chains(chains):
        chains = list(chains)
        while chains:
            nxt = []
            for c in chains:
                try:
                    next(c)
                    nxt.append(c)
                except StopIteration:
                    pass
            chains = nxt

    evac_rr = [0]

    def evac(out, in_, r, w):
        evac_rr[0] += 1
        if evac_rr[0] % 2:
            cpy('act', out, in_, r, w)
        else:
            cpy('dve', out, in_, r, w)

    slc = [0]

    def chk(k):
        if stop <= k + 10 * slc[0]:
            raise StopBuild()

    def body():
      for s in range(NSEQ):
        seqst = ExitStack()
        cosT = sb(seqst, [128, NT, 8], F32, "cosT")
        sinT = sb(seqst, [128, NT, 8], F32, "sinT")
        with ExitStack() as st:
            xtm = [sb(st, [128, DM], F32, "xtm") for _ in range(2)]
            xblk = [sb(st, [128, KC, 512], F32, "xblk") for _ in range(2)]
            xsb = Buf('xs')
            for tb in range(NB):
                xb_ = xblk[tb % 2]
                for t4 in range(4):
                    tt_ = tb * 4 + t4
                    xt = xtm[tt_ % 2]
                    dma(xt[:], x_d[s][tt_ * 128:(tt_ + 1) * 128, :], [], [xt])
                    for half in range(2):
                        pb = PS[half]
                        for c4 in range(4):
                            c = half * 4 + c4
                            tr(pb[:, c4 * 128:(c4 + 1) * 128], xt[:, c * 128:(c + 1) * 128], identF[:],
                               [xt, identF], [pb])
                        cpy('dve', xb_[:, half * 4:(half + 1) * 4, t4 * 128:(t4 + 1) * 128],
                            pb[:].rearrange("p (c t) -> p c t", t=128), [pb], [xb_])
                dma(xs_d[s][:, :, tb * 512:(tb + 1) * 512].rearrange("c p t -> p c t"), xb_[:], [xb_], [xsb])
            posi = sb(st, [16, 128], I32, "posi")
            posf = sb(st, [16, 128], F32, "posf")
            posT = sb(st, [128, 16], F32, "posT")
            ang = sb(st, [128, NT, 8], F32, "ang")
            kf = sb(st, [128, NT, 8], F32, "kf")
            ki_ = sb(st, [128, NT, 8], I32, "ki")
            dma(posi[:], pos_d[s].rearrange("(t p) -> t p", p=128), [], [posi])
            cpy('dve', posf[:], posi[:], [posi], [posf])
            tr(PS[2][:, 0:16], posf[0:16, :], identF[0:16, 0:16], [posf, identF], [PS[2]])
            cpy('dve', posT[:], PS[2][:, 0:16], [PS[2]], [posT])
            for tab, shift in ((sinT, 0.0), (cosT, PI / 2)):
                tt('dve', ang[:], posT[:].unsqueeze(2).to_broadcast([128, NT, 8]),
                   invf[:].unsqueeze(1).to_broadcast([128, NT, 8]), ALU.mult, [posT, invf], [ang])
                if shift:
                    ts('dve', ang[:], ang[:], shift, None, ALU.add, None, [ang], [ang])
                ts('dve', kf[:], ang[:], 1.0 / TWO_PI, None, ALU.mult, None, [ang], [kf])
                cpy('dve', ki_[:], kf[:], [kf], [ki_])
                cpy('dve', kf[:], ki_[:], [ki_], [kf])
                stt('dve', ang[:], kf[:], -TWO_PI, ang[:], ALU.mult, ALU.add, [kf, ang], [ang])
                ts('dve', kf[:], ang[:], PI, TWO_PI, ALU.is_gt, ALU.mult, [ang], [kf])
                tt('dve', ang[:], ang[:], kf[:], ALU.subtract, [ang, kf], [ang])
                ts('dve', kf[:], ang[:], -PI, TWO_PI, ALU.is_lt, ALU.mult, [ang], [kf])
                tt('dve', ang[:], ang[:], kf[:], ALU.add, [ang, kf], [ang])
                act(tab[:], ang[:], AF.Sin, [ang], [tab])
        S.fence()
        chk(1)

        for l in range(DEPTH):
            slc[0] = s * DEPTH + l
            last_layer = (l == DEPTH - 1)
            lay = ExitStack()
            hT = sb(lay, [128, KC, SL], BF16, "hT")
            with ExitStack() as st:
                xblk = [sb(st, [128, KC, 512], F32, "xblk") for _ in range(2)]
                sq = sb(st, [128, KC, 512], F32, "sq")
                rs = sb(st, [128, 512], F32, "rs")
                rs2 = sb(st, [128, 512], F32, "rs2")
                for tb in range(NB):
                    xb_ = xblk[tb % 2]
                    dma(xb_[:], xs_d[s][:, :, tb * 512:(tb + 1) * 512].rearrange("c p t -> p c t"), [xsb], [xb_])
                    norm_block(st, xb_, l, lambda c, tb=tb: hT[:, c, tb * 512:(tb + 1) * 512], [hT], 0, (sq, rs, rs2))
            S.fence()
            if dbg and s == 0 and l == 0:
                dma(dbg_h, hT[:], [hT], [], q='pool')
            chk(2)

            inner = ExitStack()
            brT = [sb(inner, [128, 4, SL], BF16, "brT%d" % b) for b in range(3)]

            def proj_fm(wt, col0, dst_fn, dst, pbanks):
                for tb in range(NB):
                    pb = PS[pbanks[tb % 2]]
                    for kc in range(KC):
                        mm(pb[:], wt[:, kc, col0:col0 + 128], hT[:, kc, tb * 512:(tb + 1) * 512], kc == 0, kc == KC - 1,
                           [wt, hT], [pb])
                    evac(dst_fn(tb), pb[:], [pb], [dst])

            def proj_vpad(wt, col0, vp, pbanks):
                for g4 in range(4):
                    pb = PS[pbanks[g4 % 2]]
                    for t4 in range(4):
                        tt_ = g4 * 4 + t4
                        for kc in range(KC):
                            mm(pb[:, t4 * 128:(t4 + 1) * 128], hT[:, kc, tt_ * 128:(tt_ + 1) * 128],
                               wt[:, kc, col0:col0 + 128], kc == 0, kc == KC - 1, [wt, hT], [pb])
                    pv = pb[:].rearrange("p (t f) -> p t f", f=128)
                    for hl in range(2):
                        cpy('dve', vp[hl][:, g4 * 4:(g4 + 1) * 4, hl * 64:(hl + 1) * 64], pv[:, :, hl * 64:(hl + 1) * 64],
                            [pb], [vp[hl]])

            with ExitStack() as st:
                wq = sb(st, [128, KC, 512], BF16, "wq"); wk = sb(st, [128, KC, 512], BF16, "wk")
                wv = sb(st, [128, KC, 512], BF16, "wv")
                for wt, c0 in ((wq, QA), (wk, KA), (wv, VA)):
                    wload(wt, wt[:], ('in', l), winb[l], c0, c0 + 512)
                qTp = [sb(st, [128, SL], BF16, "qTp") for _ in range(2)]
                kTp = [sb(st, [128, SL], BF16, "kTp") for _ in range(2)]
                vpd = [[sb(st, [128, NT, 128], BF16, "vpd") for _ in range(2)] for _ in range(2)]
                for a in vpd:
                    for v_ in a:
                        mset('pool', v_[:], 0.0, [v_])
                W = []
                for ch in range(2):
                    W.append(dict(e=sb(st, [128, 512], F32, "e"), sp=sb(st, [128, 512], F32, "sp"),
                                  nl=sb(st, [128, 512], BF16, "nl"), R=sb(st, [128, 512], F32, "R"),
                                  Rb=sb(st, [128, 512], BF16, "Rb"), tsum=sb(st, [128, 512], F32, "tsum"),
                                  wT=sb(st, [128, 512], BF16, "wT"), z=PS[2 + ch], lat=PS[4 + ch]))

                def chainA(hl, qT, kT, vp, Q, acc, w_):
                    base = hl * 64
                    nk = 4 * Q + 4
                    for idx, kt in enumerate(range(nk - 1, -1, -1)):
                        diag = kt >= 4 * Q
                        j = kt - 4 * Q
                        mm(w_['z'][:], kT[base:base + 64, kt * 128:(kt + 1) * 128],
                           qT[base:base + 64, Q * 512:(Q + 1) * 512], True, True, [kT, qT], [w_['z']])
                        yield
                        act(w_['e'][:], w_['z'][:], AF.Exp, [w_['z']], [w_['e']], scale=-SCALE)
                        yield
                        act(w_['sp'][:], w_['e'][:], AF.Ln, [w_['e']], [w_['sp']], bias=1.0)
                        yield
                        stt('dve', w_['nl'][:], w_['z'][:], SCALE, w_['sp'][:], ALU.mult, ALU.add,
                            [w_['z'], w_['sp']], [w_['nl']])
                        yield
                        if diag:
                            tt('pool', w_['nl'][:], w_['nl'][:], maskS[j][:], ALU.mult, [w_['nl'], maskS[j]], [w_['nl']])
                            yield
                        last_lat = (idx == 0) and not diag
                        mm(w_['lat'][:], triU[:], w_['nl'][:], True, (idx == 0 and not diag), [triU, w_['nl']], [w_['lat']])
                        if idx > 0:
                            mm(w_['lat'][:], onesB[:], w_['Rb'][:], False, not diag, [onesB, w_['Rb']], [w_['lat']])
                        if diag:
                            mm(w_['lat'][:], identB[:], posA[j][:], False, True, [identB, posA[j]], [w_['lat']])
                        yield
                        tt('dve', w_['tsum'][:], w_['lat'][:], w_['sp'][:], ALU.add, [w_['lat'], w_['sp']], [w_['tsum']])
                        yield
                        act(w_['wT'][:], w_['tsum'][:], AF.Exp, [w_['tsum']], [w_['wT']], scale=-1.0)
                        yield
                        mm(acc[:], vp[hl][:, kt, :], w_['wT'][:], hl == 0 and idx == 0, hl == 1 and idx == nk - 1,
                           [vp[hl], w_['wT']], [acc])
                        yield
                        if kt > 0:
                            if idx == 0:
                                cpy('pool', w_['R'][:], w_['nl'][:], [w_['nl']], [w_['R']])
                            else:
                                tt('pool', w_['R'][:], w_['R'][:], w_['nl'][:], ALU.add, [w_['R'], w_['nl']], [w_['R']])
                            yield
                            cpy('pool', w_['Rb'][:], w_['R'][:], [w_['R']], [w_['Rb']])
                            yield

                for hp in range(4):
                    i_ = hp % 2
                    chk(2.1)
                    proj_fm(wq, hp * 128, lambda tb: qTp[i_][:, tb * 512:(tb + 1) * 512], qTp[i_], (0, 1))
                    chk(2.15)
                    proj_fm(wk, hp * 128, lambda tb: kTp[i_][:, tb * 512:(tb + 1) * 512], kTp[i_], (0, 1))
                    chk(2.17)
                    proj_vpad(wv, hp * 128, vpd[i_], (0, 1))
                    chk(2.2)
                    for Q in range(NB):
                        acc = PS[6 + (Q % 2)]
                        run_chains([chainA(hl, qTp[i_], kTp[i_], vpd[i_], Q, acc, W[hl]) for hl in range(2)])
                        evac(brT[0][:, hp, Q * 512:(Q + 1) * 512], acc[:], [acc], [brT[0]])
                        chk(2.5)
            S.fence()
            if dbg and s == 0 and l == 0:
                dma(dbg_br[0], brT[0][:], [brT[0]], [], q='pool')
            chk(3)

            with ExitStack() as st:
                wq = sb(st, [128, KC, 512], BF16, "wq"); wk = sb(st, [128, KC, 512], BF16, "wk")
                wv = sb(st, [128, KC, 512], BF16, "wv")
                wfl = sb(st, [128, KC, 8], BF16, "wfl")
                for wt, c0 in ((wq, QB), (wk, KB), (wv, VB)):
                    wload(wt, wt[:], ('in', l), winb[l], c0, c0 + 512)
                wload(wfl, wfl[:], ('in', l), winb[l], FLC, FLC + 8)
                qTp = [sb(st, [128, SL], BF16, "qTp") for _ in range(2)]
                kTp = [sb(st, [128, SL], BF16, "kTp") for _ in range(2)]
                vpd = [[sb(st, [128, NT, 128], BF16, "vpd") for _ in range(2)] for _ in range(2)]
                for a in vpd:
                    for v_ in a:
                        mset('pool', v_[:], 0.0, [v_])
                augp = [sb(st, [3, 2, SL], BF16, "augp") for _ in range(2)]
                cnegT = sb(st, [128, NT, 8], F32, "cnegT")
                augb = Buf('augd')
                with ExitStack() as st2:
                    lfe = sb(st2, [8, SL], F32, "lfe")
                    cneg = sb(st2, [8, SL], F32, "cneg"); v8 = sb(st2, [8, SL], F32, "v8")
                    part = sb(st2, [8, SL], BF16, "part")
                    for tb in range(NB):
                        pb = PS[tb % 2]
                        for kc in range(KC):
                            mm(pb[0:8, :], wfl[:, kc, :], hT[:, kc, tb * 512:(tb + 1) * 512], kc == 0, kc == KC - 1,
                               [wfl, hT], [pb])
                        act(lfe[:, tb * 512:(tb + 1) * 512], pb[0:8, :], AF.Exp, [pb, bfneg], [lfe],
                            bias=bfneg[:, l:l + 1], scale=-1.0)
                    act(lfe[:], lfe[:], AF.Ln, [lfe], [lfe], bias=1.0)
                    S.op('dve', lambda e: e.tensor_tensor_scan(out=cneg[:], data0=lfe[:], data1=lfe[:], initial=0.0,
                                                               op0=ALU.add, op1=ALU.bypass), bl([lfe]), bl([cneg]))
                    ts('dve', v8[:], cneg[:], -8.0, None, ALU.mult, None, [cneg], [v8])
                    for pi_ in range(3):
                        cpy('dve', part[:], v8[:], [v8], [part])
                        if pi_ < 2:
                            tt('dve', v8[:], v8[:], part[:], ALU.subtract, [v8, part], [v8])
                        dma(aug_d[pi_], part[:], [part], [augb])
                    for tt_ in range(NT):
                        tr(PS[2][:, tt_ * 8:(tt_ + 1) * 8], cneg[0:8, tt_ * 128:(tt_ + 1) * 128], identF[0:8, 0:8],
                           [cneg, identF], [PS[2]])
                    cpy('dve', cnegT[:], PS[2][:, 0:128].rearrange("p (t h) -> p t h", h=8), [PS[2]], [cnegT])
                S.fence()
                W = []
                for ch in range(2):
                    W.append(dict(pT=sb(st, [128, 512], BF16, "pT"), z=PS[2 + ch]))

                def chainB(hl, h, qT, kT, vp, aq, Q, num, den, w_):
                    base = hl * 64
                    nk = 4 * Q + 4
                    for idx, kt in enumerate(range(nk)):
                        diag = kt >= 4 * Q
                        j = kt - 4 * Q
                        mm(w_['z'][:], kT[base:base + 64, kt * 128:(kt + 1) * 128],
                           qT[base:base + 64, Q * 512:(Q + 1) * 512], True, False, [kT, qT], [w_['z']])
                        mm(w_['z'][:], ones3[0:3, :], aq[0:3, hl, Q * 512:(Q + 1) * 512], False, not diag,
                           [ones3, aq], [w_['z']])
                        if diag:
                            mm(w_['z'][:], identB[:], negB[j][:], False, True, [identB, negB[j]], [w_['z']])
                        yield
                        act(w_['pT'][:], w_['z'][:], AF.Exp, [w_['z'], cnegT], [w_['pT']],
                            bias=cnegT[:, kt, h:h + 1], scale=SCALE)
                        yield
                        first = (hl == 0 and idx == 0)
                        lastf = (hl == 1 and idx == nk - 1)
                        mm(num[:], vp[hl][:, kt, :], w_['pT'][:], first, lastf, [vp[hl], w_['pT']], [num])
                        mm(den[:], opad[hl][:], w_['pT'][:], first, lastf, [opad[hl], w_['pT']], [den])
                        yield

                for hp in range(4):
                    i_ = hp % 2
                    proj_fm(wq, hp * 128, lambda tb: qTp[i_][:, tb * 512:(tb + 1) * 512], qTp[i_], (0, 1))
                    proj_fm(wk, hp * 128, lambda tb: kTp[i_][:, tb * 512:(tb + 1) * 512], kTp[i_], (0, 1))
                    proj_vpad(wv, hp * 128, vpd[i_], (0, 1))
                    dma(augp[i_][:], aug_d[:, 2 * hp:2 * hp + 2, :], [augb], [augp[i_]])
                    for Q in range(NB):
                        num = PS[4 + 2 * (Q % 2)]
                        den = PS[5 + 2 * (Q % 2)]
                        run_chains([chainB(hl, 2 * hp + hl, qTp[i_], kTp[i_], vpd[i_], augp[i_], Q, num, den, W[hl])
                                    for hl in range(2)])
                        S.op('dve', lambda e, den=den: e.reciprocal(out=rden[:], in_=den[:]), bl([den]), bl([rden]))
                        tt('dve', brT[1][:, hp, Q * 512:(Q + 1) * 512], num[:], rden[:], ALU.mult, [num, rden], [brT[1]])
            S.fence()
            if dbg and s == 0 and l == 0:
                dma(dbg_br[1], brT[1][:], [brT[1]], [], q='pool')
            chk(4)

            with ExitStack() as st:
                qcT = sb(st, [128, 4, SL], BF16, "qcT")
                kcTd = sb(st, [128, 2, SL], BF16, "kcTd")
                qiT = sb(st, [128, 2, SL], BF16, "qiT")
                kiT2 = sb(st, [128, SL], BF16, "kiT2")
                vpc = [[sb(st, [128, NT, 128], BF16, "vpc") for _ in range(2)] for _ in range(2)]
                wiT = sb(st, [128, NT, 4], F32, "wiT")
                for a in vpc:
                    for v_ in a:
                        mset('pool', v_[:], 0.0, [v_])
                with ExitStack() as st2:
                    wc = sb(st2, [128, KC, CW], BF16, "wc")
                    wload(wc, wc[:], ('in', l), winb[l], CC, CC + CW)
                    cq = sb(st2, [128, 512], F32, "cq")
                    c1 = sb(st2, [128, 512], F32, "c1")
                    c2 = sb(st2, [128, 128], F32, "c2")
                    kd = sb(st2, [128, 128], F32, "kd")
                    kcd = sb(st2, [128, 2, 128], F32, "kcd")
                    rt = [sb(st2, [128, 8, 8], F32, "rt") for _ in range(4)]

                    def rope(t_, ncol, nh, tt_):
                        dv = t_[:, 0:ncol].rearrange("p (h d) -> p h d", d=64)
                        cs = cosT[:, tt_, :].unsqueeze(1).to_broadcast([128, nh, 8])
                        sn = sinT[:, tt_, :].unsqueeze(1).to_broadcast([128, nh, 8])
                        x1 = dv[:, :, 0:8]
                        x2 = dv[:, :, 8:16]
                        r0, r1, r2, r3 = [r_[:, 0:nh, :] for r_ in rt]
                        tt('dve', r0, x1, cs, ALU.mult, [t_, cosT], [rt[0]])
                        tt('dve', r1, x2, sn, ALU.mult, [t_, sinT], [rt[1]])
                        tt('dve', r2, x2, cs, ALU.mult, [t_, cosT], [rt[2]])
                        tt('dve', r3, x1, sn, ALU.mult, [t_, sinT], [rt[3]])
                        tt('dve', x1, r0, r1, ALU.subtract, [rt[0], rt[1], t_], [t_])
                        tt('dve', x2, r2, r3, ALU.add, [rt[2], rt[3], t_], [t_])

                    for tt_ in range(NT):
                        tsl = slice(tt_ * 128, (tt_ + 1) * 128)
                        for bi, (c0, cw_) in enumerate(((0, 512), (512, 512), (1024, 68))):
                            pb = PS[bi]
                            for kc in range(KC):
                                mm(pb[:, 0:cw_], hT[:, kc, tsl], wc[:, kc, c0:c0 + cw_], kc == 0, kc == KC - 1,
                                   [wc, hT], [pb])
                        cpy('act', cq[:], PS[0][:], [PS[0]], [cq])
                        cpy('act', c1[:], PS[1][:], [PS[1]], [c1])
                        cpy('dve', c2[:, 0:68], PS[2][:, 0:68], [PS[2]], [c2])
                        rope(cq, 512, 8, tt_)
                        rope(c1, 512, 8, tt_)
                        rope(c2, 64, 1, tt_)
                        cpy('pool', kd[:, 0:64], c2[:, 0:64], [c2], [kd])
                        cpy('pool', kd[:, 64:128], c2[:, 0:64], [c2], [kd])
                        ts('dve', wiT[:, tt_, :], c2[:, 64:68], 0.5, None, ALU.mult, None, [c2], [wiT])
                        for g in range(2):
                            for half in range(2):
                                cpy('dve', vpc[g][half][:, tt_, half * 64:(half + 1) * 64],
                                    PS[1][:, 128 + g * 64:128 + (g + 1) * 64], [PS[1]], [vpc[g][half]])
                                cpy('pool', kcd[:, g, half * 64:(half + 1) * 64], c1[:, g * 64:(g + 1) * 64],
                                    [c1], [kcd])
                        for c in range(4):
                            tr(PS[3][:, c * 128:(c + 1) * 128], cq[:, c * 128:(c + 1) * 128], identF[:],
                               [cq, identF], [PS[3]])
                        cpy('dve', qcT[:, :, tsl], PS[3][:].rearrange("p (c t) -> p c t", t=128), [PS[3]], [qcT])
                        for g in range(2):
                            tr(PS[4][:, g * 128:(g + 1) * 128], kcd[:, g, :], identF[:], [kcd, identF], [PS[4]])
                        for c in range(2):
                            tr(PS[4][:, (2 + c) * 128:(3 + c) * 128], c1[:, 256 + c * 128:256 + (c + 1) * 128], identF[:],
                               [c1, identF], [PS[4]])
                        tr(PS[5][:, 0:128], kd[:], identF[:], [kd, identF], [PS[5]])
                        cpy('dve', kcTd[:, :, tsl], PS[4][:, 0:256].rearrange("p (c t) -> p c t", t=128), [PS[4]], [kcTd])
                        cpy('dve', qiT[:, :, tsl], PS[4][:, 256:512].rearrange("p (c t) -> p c t", t=128), [PS[4]], [qiT])
                        cpy('dve', kiT2[:, tsl], PS[5][:, 0:128], [PS[5]], [kiT2])
                S.fence()
                chk(4.3)
                with ExitStack() as st3:
                    sc = [sb(st3, [128, SL], F32, "sc") for _ in range(2)]
                    junk = sb(st3, [128, SL], BF16, "junk")
                    mk = [sb(st3, [128, SL], F32, "mk") for _ in range(1)]
                    mkT = [sb(st3, [128, NT, 512], BF16, "mkT") for _ in range(1)]
                    rl = [sb(st3, [128, 512], F32, "rl") for _ in range(2)]
                    mx = sb(st3, [128, 1], F32, "mx"); mn = sb(st3, [128, 1], F32, "mn")
                    wd = sb(st3, [128, 1], F32, "wd")
                    hs = sb(st3, [128, NBIS + 1], F32, "hs")
                    mids = sb(st3, [128, NBIS + 1], F32, "mids")
                    cnts = sb(st3, [128, NBIS], F32, "cnts")
                    gg = sb(st3, [128, 1], F32, "gg")
                    W = []
                    for ch in range(2):
                        W.append(dict(pT=sb(st3, [128, 512], BF16, "pT"), pm=sb(st3, [128, 512], BF16, "pm"),
                                      z=PS[4 + ch]))
                    rlc = [0]

                    def indexer(i):
                        Q = i // 4
                        j = i % 4
                        n = (i + 1) * 128
                        nfull = (4 * Q + 4) * 128
                        sc_ = sc[i % 2]
                        mk_ = mk[0]
                        qsl = slice(i * 128, (i + 1) * 128)
                        for c0 in range(0, n, 512):
                            cw_ = min(512, n - c0)
                            for h in range(4):
                                pb = PS[rlc[0] % 2]
                                r_ = rl[rlc[0] % 2]
                                rlc[0] += 1
                                hb = (h % 2) * 64
                                mm(pb[:, 0:cw_], qiT[hb:hb + 64, h // 2, qsl], kiT2[hb:hb + 64, c0:c0 + cw_], True, True,
                                   [qiT, kiT2], [pb])
                                act(r_[:, 0:cw_], pb[:, 0:cw_], AF.Relu, [pb], [r_], scale=0.125)
                                if h == 0:
                                    ts('dve', sc_[:, c0:c0 + cw_], r_[:, 0:cw_], wiT[:, i, 0:1], None, ALU.mult, None,
                                       [r_, wiT], [sc_])
                                else:
                                    stt('dve', sc_[:, c0:c0 + cw_], r_[:, 0:cw_], wiT[:, i, h:h + 1], sc_[:, c0:c0 + cw_],
                                        ALU.mult, ALU.add, [r_, wiT, sc_], [sc_])
                        if i >= 2:
                            S.op('dve', lambda e: e.tensor_reduce(out=mx[:], in_=sc_[:, 0:n], axis=AX.X, op=ALU.max),
                                 bl([sc_]), bl([mx]))
                            S.op('dve', lambda e: e.tensor_reduce(out=mn[:], in_=sc_[:, 0:n], axis=AX.X, op=ALU.min),
                                 bl([sc_]), bl([mn]))
                        tt('pool', sc_[:, qsl], sc_[:, qsl], cmask[:], ALU.add, [sc_, cmask], [sc_])
                        if n < nfull:
                            mset('pool', sc_[:, n:nfull], NEG, [sc_])
                        if i >= 2:
                            stt('dve', wd[:], mx[:], 1.0, mn[:], ALU.add, ALU.subtract, [mx, mn], [wd])
                            ts('dve', hs[:], pow2[:], wd[:, 0:1], None, ALU.mult, None, [pow2, wd], [hs])
                            tt('dve', mids[:, 0:1], mn[:], hs[:, 0:1], ALU.add, [mn, hs], [mids])
                            mset('dve', cnts[:], 0.0, [cnts])
                            for k in range(NBIS):
                                ts('dve', junk[:, 0:n], sc_[:, 0:n], mids[:, k:k + 1], 0.0, ALU.is_ge, ALU.add,
                                   [sc_, mids], [junk, cnts], accum=cnts[:, k:k + 1])
                                si = k + 1 if k < NBIS - 1 else k
                                ts('dve', gg[:], cnts[:, k:k + 1], 255.5, hs[:, k:k + 1], ALU.is_gt, ALU.mult,
                                   [cnts, hs], [gg])
                                stt('dve', mids[:, k + 1:k + 2], gg[:], hs[:, si:si + 1], mids[:, k:k + 1],
                                    ALU.subtract, ALU.add, [gg, hs, mids], [mids])
                            tau = mids[:, NBIS:NBIS + 1]
                            taub = mids
                        else:
                            tau = tauc[:, 0:1]
                            taub = tauc
                        ts('dve', mk_[:, 0:nfull], sc_[:, 0:nfull], tau, None, ALU.is_ge, None, [sc_, taub], [mk_])
                        mt = mkT[0]
                        nkt = 4 * Q + 4
                        for k0 in range(0, nkt, 4):
                            kn = min(4, nkt - k0)
                            pbi = 2 + ((k0 // 4) % 2)
                            for kk in range(kn):
                                kt = k0 + kk
                                tr(PS[pbi][:, kk * 128:(kk + 1) * 128], mk_[:, kt * 128:(kt + 1) * 128], identF[:],
                                   [mk_, identF], [PS[pbi]])
                            cpy('dve', mt[:, k0:k0 + kn, j * 128:(j + 1) * 128],
                                PS[pbi][:, 0:kn * 128].rearrange("p (k t) -> p k t", t=128), [PS[pbi]], [mt])

                    def chainC(head, Q, num, den, w_, first_chain, last_chain):
                        g = head // 4
                        half = head % 2
                        chunk = head // 2
                        base = half * 64
                        nk = 4 * Q + 4
                        mt = mkT[0]
                        for idx, kt in enumerate(range(nk)):
                            mm(w_['z'][:], kcTd[base:base + 64, g, kt * 128:(kt + 1) * 128],
                               qcT[base:base + 64, chunk, Q * 512:(Q + 1) * 512], True, True, [kcTd, qcT], [w_['z']])
                            yield
                            act(w_['pT'][:], w_['z'][:], AF.Exp, [w_['z']], [w_['pT']], scale=SCALE)
                            yield
                            tt('dve', w_['pm'][:], w_['pT'][:], mt[:, kt, :], ALU.mult, [w_['pT'], mt], [w_['pm']])
                            yield
                            first = first_chain and idx == 0
                            lastf = last_chain and idx == nk - 1
                            mm(num[:], vpc[g][half][:, kt, :], w_['pm'][:], first, lastf, [vpc[g][half], w_['pm']], [num])
                            mm(den[:], opad[half][:], w_['pm'][:], first, lastf, [opad[half], w_['pm']], [den])
                            yield

                    for Q in range(NB):
                        for j in range(4):
                            indexer(4 * Q + j)
                            chk(4.4 if j < 2 else 4.5)
                        chk(4.6)
                        for hp in range(4):
                            num = PS[6]
                            den = PS[7]
                            run_chains([chainC(2 * hp + hl, Q, num, den, W[hl], hl == 0, hl == 1) for hl in range(2)])
                            S.op('dve', lambda e, den=den: e.reciprocal(out=rden[:], in_=den[:]), bl([den]), bl([rden]))
                            tt('dve', brT[2][:, hp, Q * 512:(Q + 1) * 512], num[:], rden[:], ALU.mult, [num, rden], [brT[2]])
                            chk(4.7)
            S.fence()
            if dbg and s == 0 and l == 0:
                dma(dbg_br[2], brT[2][:], [brT[2]], [], q='pool')
            chk(5)

            with ExitStack() as st:
                yT = sb(st, [128, KC, SL], BF16, "yT")
                wb_ = [sb(st, [128, 4, 512], BF16, "wb") for _ in range(3)]
                wg_ = [sb(st, [128, KC, 512], BF16, "wg") for _ in range(3)]
                sg = [sb(st, [128, 512], F32, "sg") for _ in range(2)]
                accy = sb(st, [128, 512], F32, "accy")
                tmpy = sb(st, [128, 512], F32, "tmpy")
                pc_ = [0]
                for fg in range(2):
                    for b in range(3):
                        dma(wb_[b][:], wbrb[l][b][:, fg * 512:(fg + 1) * 512].rearrange("(kc p) f -> p kc f", p=128),
                            [wbuf[('br', l)]], [wb_[b]])
                        wload(wg_[b], wg_[b][:], ('in', l), winb[l], GATE + b * DM + fg * 512, GATE + b * DM + (fg + 1) * 512)
                    for tb in range(NB):
                        tsl = slice(tb * 512, (tb + 1) * 512)
                        for fc in range(4):
                            c = fg * 4 + fc
                            fsl = slice(fc * 128, (fc + 1) * 128)
                            for b in range(3):
                                pp = PS[(pc_[0] % 4) * 2]
                                pg = PS[(pc_[0] % 4) * 2 + 1]
                                sg_ = sg[pc_[0] % 2]
                                pc_[0] += 1
                                for kc in range(KC):
                                    mm(pg[:], wg_[b][:, kc, fsl], hT[:, kc, tsl], kc == 0, kc == KC - 1, [wg_[b], hT], [pg])
                                for k4 in range(4):
                                    mm(pp[:], wb_[b][:, k4, fsl], brT[b][:, k4, tsl], k4 == 0, k4 == 3, [wb_[b], brT[b]], [pp])
                                act(sg_[:], pg[:], AF.Sigmoid, [pg], [sg_])
                                if b == 0:
                                    tt('dve', accy[:], pp[:], sg_[:], ALU.mult, [pp, sg_], [accy])
                                elif b == 1:
                                    tt('dve', tmpy[:], pp[:], sg_[:], ALU.mult, [pp, sg_], [tmpy])
                                    tt('pool', accy[:], accy[:], tmpy[:], ALU.add, [accy, tmpy], [accy])
                                else:
                                    tt('dve', tmpy[:], pp[:], sg_[:], ALU.mult, [pp, sg_], [tmpy])
                                    tt('pool', yT[:, c, tsl], accy[:], tmpy[:], ALU.add, [accy, tmpy], [yT])
                S.fence()
                for c in range(KC):
                    cpy('pool' if c % 2 else 'dve', hT[:, c, :], yT[:, c, :], [yT], [hT])
            S.fence()
            inner.close()
            yTT = hT
            chk(6)

            with ExitStack() as st:
                wo = sb(st, [128, KC, DM], BF16, "wo")
                wload(wo, wo[:, :, 0:512], ('out', l), woutb[l], 0, 512)
                wload(wo, wo[:, :, 512:1024], ('out', l), woutb[l], 512, 1024)
                xblk = [sb(st, [128, KC, 512], F32, "xblk") for _ in range(1)]
                sq = sb(st, [128, KC, 512], F32, "sq")
                rs = sb(st, [128, 512], F32, "rs")
                rs2 = sb(st, [128, 512], F32, "rs2")
                h2T = sb(st, [128, KC, 512], BF16, "h2T")
                uT = sb(st, [128, 32, 512], BF16, "uT")
                wu = [sb(st, [128, KC, 512], BF16, "wu") for _ in range(2)]
                wd_ = [sb(st, [128, 32, 256], BF16, "wd") for _ in range(1)]
                rl = [sb(st, [128, 512], F32, "rl") for _ in range(2)]
                otm = [sb(st, [128, DM], F32, "otm") for _ in range(2)] if last_layer else None
                fin = sb(st, [128, KC, 512], F32, "fin") if last_layer else None
                pcn = [0]
                xsb2 = Buf('xs2')
                for tb in range(NB):
                    tsl = slice(tb * 512, (tb + 1) * 512)
                    xb_ = xblk[0]
                    dma(xb_[:], xs_d[s][:, :, tsl].rearrange("c p t -> p c t"), [xsb], [xb_])
                    for fc in range(KC):
                        pb = PS[pcn[0] % 2]
                        pcn[0] += 1
                        for kc in range(KC):
                            mm(pb[:], wo[:, kc, fc * 128:(fc + 1) * 128], yTT[:, kc, tsl], kc == 0, kc == KC - 1,
                               [wo, yTT], [pb])
                        tt('dve', xb_[:, fc, :], pb[:], xb_[:, fc, :], ALU.add, [pb, xb_], [xb_])
                    norm_block(st, xb_, 2 + l, lambda c: h2T[:, c, :], [h2T], 2, (sq, rs, rs2))
                    for fg in range(8):
                        wu_ = wu[fg % 2]
                        wload(wu_, wu_[:], ('up', l), wupb[l], fg * 512, (fg + 1) * 512)
                        for fc in range(4):
                            pb = PS[pcn[0] % 2]
                            r_ = rl[pcn[0] % 2]
                            pcn[0] += 1
                            for kc in range(KC):
                                mm(pb[:], wu_[:, kc, fc * 128:(fc + 1) * 128], h2T[:, kc, :], kc == 0, kc == KC - 1,
                                   [wu_, h2T], [pb])
                            act(r_[:], pb[:], AF.Relu, [pb], [r_])
                            tt('pool', uT[:, fg * 4 + fc, :], r_[:], r_[:], ALU.mult, [r_], [uT])
                    for f2 in range(4):
                        wdt = wd_[0]
                        dma(wdt[:], wdnb[l][:, f2 * 256:(f2 + 1) * 256].rearrange("(kc p) f -> p kc f", p=128),
                            [wbuf[('dn', l)]], [wdt])
                        for fc in range(2):
                            c = f2 * 2 + fc
                            pb = PS[3 + (pcn[0] % 2)]
                            pcn[0] += 1
                            for kc in range(32):
                                mm(pb[:], wdt[:, kc, fc * 128:(fc + 1) * 128], uT[:, kc, :], kc == 0, kc == 31,
                                   [wdt, uT], [pb])
                            tt('dve', xb_[:, c, :], pb[:], xb_[:, c, :], ALU.add, [pb, xb_], [xb_])
                    if dbg and s == 0 and l == 0:
                        dma(dbg_x[:, :, tsl].rearrange("c p t -> p c t"), xb_[:], [xb_], [])
                    if not last_layer:
                        dma(xs_d[s][:, :, tsl].rearrange("c p t -> p c t"), xb_[:], [xb_], [xsb2])
                    else:
                        if final:
                            norm_block(st, xb_, 4, lambda c: fin[:, c, :], [fin], 2, (sq, rs, rs2))
                        else:
                            for c in range(KC):
                                cpy('pool' if c % 2 else 'dve', fin[:, c, :], xb_[:, c, :], [xb_], [fin])
                        for t4 in range(4):
                            ot = otm[t4 % 2]
                            for half in range(2):
                                pb = PS[5 + half]
                                for c4 in range(4):
                                    c = half * 4 + c4
                                    tr(pb[:, c4 * 128:(c4 + 1) * 128], fin[:, c, t4 * 128:(t4 + 1) * 128], identF[:],
                                       [fin, identF], [pb])
                                evac(ot[:, half * 512:(half + 1) * 512], pb[:], [pb], [ot])
                            tt_ = tb * 4 + t4
                            dma(out_d[s][tt_ * 128:(tt_ + 1) * 128, :], ot[:], [ot], [])
                xsb = xsb2
            S.fence()
            lay.close()
        seqst.close()
        S.fence()

    try:
        body()
    except StopBuild:
        S.fence()
    S.emit(top)
    top.close()
    return nc, S


_CACHE = {}


def kernel(x, positions, g_mix, w_in, b_forget, w_branch, w_out, g_mlp, w_up, w_down, g_final):
    n = 8
    if 'nc' not in _CACHE:
        _CACHE['nc'] = build(2, 2)[0]
    nc = _CACHE['nc']
    f32 = lambda a: np.ascontiguousarray(np.asarray(a, dtype=np.float32))
    x = f32(x)
    pos = np.ascontiguousarray(np.asarray(positions, dtype=np.int32))
    shared = dict(g_mix=f32(g_mix), w_in=f32(w_in), b_forget=f32(b_forget), w_branch=f32(w_branch),
                  w_out=f32(w_out), g_mlp=f32(g_mlp), w_up=f32(w_up), w_down=f32(w_down), g_final=f32(g_final))
    in_maps = []
    for c in range(n):
        m = dict(shared)
        m['x'] = np.ascontiguousarray(x[2 * c:2 * c + 2])
        m['positions'] = np.ascontiguousarray(pos[2 * c:2 * c + 2])
        in_maps.append(m)
    res = run_bass_kernel_spmd(nc, in_maps, core_ids=list(range(n)))
    return np.concatenate([np.asarray(r["out"], dtype=np.float32) for r in res.results], axis=0)
```

```python
import numpy as np
from contextlib import ExitStack
import concourse.bass as bass
import concourse.mybir as mybir
from concourse.bass_utils import run_bass_kernel_spmd

F32 = mybir.dt.float32
BF16 = mybir.dt.bfloat16
I32 = mybir.dt.int32
U32 = mybir.dt.uint32
AF = mybir.ActivationFunctionType
ALU = mybir.AluOpType
AX = mybir.AxisListType

CE = ('pe', 'act', 'dve', 'pool')
NCE = len(CE)
EPOCH = 4000
NDSEM = 12


class Buf:
    __slots__ = ('name', 'w', 'rs')

    def __init__(self, name=''):
        self.name = name
        self.w = None
        self.rs = []


class Op:
    __slots__ = ('eng', 'q', 'fn', 'idx', 'deps', 'waits', 'marked', 'semi', 'semv', 'clock', 'isdma', 'dnum')


class Sch:
    def __init__(self, nc):
        self.nc = nc
        self.ops = []
        self.cnt = {e: 0 for e in CE}
        self.last = {e: None for e in CE}
        self.ndma = {'sync': 0, 'pool': 0, 'act': 0}
        self.dmas = {'sync': [], 'pool': [], 'act': []}

    def _rec(self, o, reads, writes):
        deps = {}
        for b in reads:
            if b.w is not None:
                deps[id(b.w)] = b.w
        for b in writes:
            if b.w is not None:
                deps[id(b.w)] = b.w
            for r in b.rs:
                deps[id(r)] = r
        deps.pop(id(o), None)
        o.deps = list(deps.values())
        for b in reads:
            b.rs.append(o)
        for b in writes:
            b.w = o
            b.rs = []
        self.ops.append(o)

    def op(self, eng, fn, reads=(), writes=()):
        o = Op()
        o.eng = eng; o.q = eng; o.fn = fn; o.isdma = False
        self.cnt[eng] += 1
        o.idx = self.cnt[eng]
        o.marked = False
        self._rec(o, reads, writes)
        self.last[eng] = o
        return o

    def dma(self, fn, reads=(), writes=(), q='sync'):
        o = Op()
        o.eng = 'dma'; o.q = q; o.fn = fn; o.isdma = True
        o.dnum = self.ndma[q]
        self.ndma[q] += 1
        self.dmas[q].append(o)
        o.idx = 0
        o.marked = True
        self._rec(o, reads, writes)
        return o

    def fence(self):
        lst = [self.last[e] for e in CE if self.last[e] is not None]
        dl = []
        for q in self.dmas:
            dl += self.dmas[q][-NDSEM:]
        for e in CE + ('sync',):
            o = Op()
            o.eng = e; o.q = e; o.fn = None; o.isdma = False
            o.idx = 0
            o.marked = False
            o.deps = [d for d in lst if d.eng != e] + list(dl)
            self.ops.append(o)

    def plan(self):
        ei = {e: i for i, e in enumerate(CE)}
        run = {q: [0] * NCE for q in ('pe', 'act', 'dve', 'pool', 'sync')}
        seen_dma = {q: set() for q in run}
        for o in self.ops:
            q = o.q
            rc = run[q]
            waits = []
            for d in o.deps:
                if d.isdma:
                    if id(d) in seen_dma[q]:
                        continue
                    seen_dma[q].add(id(d))
                    waits.append(d)
                    dc = d.clock
                    for i in range(NCE):
                        if dc[i] > rc[i]:
                            rc[i] = dc[i]
                else:
                    if d.eng == 'pe' and o.eng == 'pe':
                        continue
                    j = ei[d.eng]
                    if rc[j] >= d.idx:
                        continue
                    waits.append(d)
                    d.marked = True
                    dc = d.clock
                    for i in range(NCE):
                        if dc[i] > rc[i]:
                            rc[i] = dc[i]
            best = {}
            fin = []
            for d in waits:
                if d.isdma:
                    fin.append(d)
                elif d.eng not in best or best[d.eng].idx < d.idx:
                    best[d.eng] = d
            fin.extend(best.values())
            o.waits = fin
            o.clock = list(rc)
            if not o.isdma and o.fn is not None:
                o.clock[ei[o.eng]] = o.idx

    def emit(self, stack):
        nc = self.nc
        self.plan()
        sems = {}

        def getsem(name):
            if name not in sems:
                sems[name] = stack.enter_context(nc.semaphore(name))
            return sems[name]

        mcount = {e: 0 for e in CE}
        for o in self.ops:
            if o.isdma:
                k = o.dnum % NDSEM
                o.semi = 'd_%s_%d' % (o.q, k)
                o.semv = 16 * (o.dnum // NDSEM + 1)
            elif o.marked:
                mcount[o.eng] += 1
                ep = (mcount[o.eng] - 1) // EPOCH
                o.semi = 'c_%s_%d' % (o.eng, ep)
                o.semv = mcount[o.eng] - ep * EPOCH
        for o in self.ops:
            if o.isdma or o.marked:
                getsem(o.semi)
        byq = {q: [] for q in ('pe', 'act', 'dve', 'pool', 'sync')}
        for o in self.ops:
            byq[o.q].append(o)
        self.nwaits = 0

        def run_queue(eng, lst):
            ring = {}
            for o in lst:
                for d in o.waits:
                    eng.wait_ge(sems[d.semi], d.semv)
                    self.nwaits += 1
                if o.isdma:
                    k = o.dnum % NDSEM
                    if k in ring:
                        p = ring[k]
                        eng.wait_ge(sems[p.semi], p.semv)
                    ring[k] = o
                    o.fn(eng).then_inc(sems[o.semi], 16)
                elif o.fn is not None:
                    ins = o.fn(eng)
                    if o.marked:
                        ins.then_inc(sems[o.semi], 1)

        with nc.Block() as block:
            @block.sync
            def _(e):
                run_queue(e, byq['sync'])
                for q in ('sync', 'pool', 'act'):
                    for o in self.dmas[q][-NDSEM:]:
                        e.wait_ge(sems[o.semi], o.semv)

            @block.tensor
            def _(e):
                run_queue(e, byq['pe'])

            @block.scalar
            def _(e):
                run_queue(e, byq['act'])

            @block.vector
            def _(e):
                run_queue(e, byq['dve'])

            @block.gpsimd
            def _(e):
                run_queue(e, byq['pool'])


SL = 2048
DM = 1024
NT = 16
NB = 4
KC = 8
QA, KA, VA, QB, KB, VB, FLC, CC, GATE = 0, 512, 1024, 1536, 2048, 2560, 3072, 3080, 4172
NCOL = 7244
CW = 1092
SCALE = 0.125
NEG = -1.0e30
NBIS = 22
TWO_PI = float(2 * np.pi)
PI = float(np.pi)


class Tl:
    def __init__(self, t, name=''):
        self.t = t
        self.b = Buf(name)

    def __getitem__(self, k):
        return self.t[k]


class StopBuild(Exception):
    pass


def build(NSEQ=2, DEPTH=2, dbg=False, stop=99, final=True):
    return _build(NSEQ, DEPTH, dbg, stop, final)


def _build(NSEQ, DEPTH, dbg, stop, final=True):
    nc = bass.Bass("TRN2", target_bir_lowering=False)

    def dram(name, shape, dtype, kind):
        return nc.dram_tensor(name, shape, dtype, kind=kind).ap()

    x_d = dram("x", [NSEQ, SL, DM], F32, "ExternalInput")
    pos_d = dram("positions", [NSEQ, SL], I32, "ExternalInput")
    gmix_d = dram("g_mix", [DEPTH, DM], F32, "ExternalInput")
    win_d = dram("w_in", [DEPTH, DM, NCOL], F32, "ExternalInput")
    bf_d = dram("b_forget", [DEPTH, 8], F32, "ExternalInput")
    wbr_d = dram("w_branch", [DEPTH, 3, 512, DM], F32, "ExternalInput")
    wout_d = dram("w_out", [DEPTH, DM, DM], F32, "ExternalInput")
    gmlp_d = dram("g_mlp", [DEPTH, DM], F32, "ExternalInput")
    wup_d = dram("w_up", [DEPTH, DM, 4 * DM], F32, "ExternalInput")
    wdn_d = dram("w_down", [DEPTH, 4 * DM, DM], F32, "ExternalInput")
    gfin_d = dram("g_final", [DM], F32, "ExternalInput")
    out_d = dram("out", [NSEQ, SL, DM], F32, "ExternalOutput")
    winb = dram("winb", [2, DM, NCOL], BF16, "Internal")
    wbrb = dram("wbrb", [2, 3, 512, DM], BF16, "Internal")
    woutb = dram("woutb", [2, DM, DM], BF16, "Internal")
    wupb = dram("wupb", [2, DM, 4 * DM], BF16, "Internal")
    wdnb = dram("wdnb", [2, 4 * DM, DM], BF16, "Internal")
    xs_d = dram("xs", [NSEQ, KC, 128, SL], F32, "Internal")
    aug_d = dram("augd", [3, 8, SL], BF16, "Internal")
    if dbg:
        dbg_h = dram("dbg_h", [128, KC, SL], F32, "ExternalOutput")
        dbg_br = dram("dbg_br", [3, 128, 4, SL], F32, "ExternalOutput")
        dbg_x = dram("dbg_x", [KC, 128, SL], F32, "ExternalOutput")

    top = ExitStack()
    S = Sch(nc)
    uid = [0]

    cur = [16384 + 256]
    peak = [0]
    LIMIT = 229376

    def sb(st, shape, dtype, name=None):
        uid[0] += 1
        nm = "%s_%d" % (name or 't', uid[0])
        isz = 2 if dtype == BF16 else 4
        nb = int(np.prod(shape[1:])) * isz
        nb = (nb + 63) // 64 * 64
        off = cur[0]
        cur[0] += nb
        peak[0] = max(peak[0], cur[0])
        assert cur[0] <= LIMIT, ("SBUF overflow", nm, cur[0])
        st.callback(lambda off=off: cur.__setitem__(0, off))
        return Tl(nc.alloc_sbuf_tensor_at(nm, shape, dtype, offset=off), nm)

    def bl(ts):
        return [t.b if isinstance(t, Tl) else t for t in ts]

    def mm(out, lhsT, rhs, start, stop, r, w):
        S.op('pe', lambda e: e.matmul(out, lhsT=lhsT, rhs=rhs, start=start, stop=stop), bl(r), bl(w))

    def tr(out, in_, ident, r, w):
        S.op('pe', lambda e: e.transpose(out=out, in_=in_, identity=ident), bl(r), bl(w))

    def act(out, in_, func, r, w, bias=None, scale=None):
        kw = {}
        if bias is not None:
            kw['bias'] = bias
        if scale is not None:
            kw['scale'] = scale
        S.op('act', lambda e: e.activation(out=out, in_=in_, func=func, **kw), bl(r), bl(w))

    def tt(eng, out, in0, in1, op, r, w):
        S.op(eng, lambda e: e.tensor_tensor(out=out, in0=in0, in1=in1, op=op), bl(r), bl(w))

    def ts(eng, out, in0, s1, s2, op0, op1, r, w, accum=None):
        kw = {}
        if op1 is not None:
            kw['op1'] = op1
        if accum is not None:
            kw['accum_out'] = accum
        S.op(eng, lambda e: e.tensor_scalar(out=out, in0=in0, scalar1=s1, scalar2=s2, op0=op0, **kw), bl(r), bl(w))

    def stt(eng, out, in0, scalar, in1, op0, op1, r, w):
        S.op(eng, lambda e: e.scalar_tensor_tensor(out=out, in0=in0, scalar=scalar, in1=in1, op0=op0, op1=op1), bl(r), bl(w))

    def cpy(eng, out, in_, r, w):
        if eng == 'act':
            S.op('act', lambda e: e.activation(out=out, in_=in_, func=AF.Copy), bl(r), bl(w))
        else:
            S.op(eng, lambda e: e.tensor_copy(out=out, in_=in_), bl(r), bl(w))

    def mset(eng, ap, val, w):
        S.op(eng, lambda e: e.memset(ap, val), [], bl(w))

    def asel(out, in_, pattern, cmp, fill, base, cm, r, w):
        S.op('pool', lambda e: e.affine_select(out=out, in_=in_, pattern=pattern, compare_op=cmp, fill=fill,
                                               base=base, channel_multiplier=cm), bl(r), bl(w))

    def dma(out, in_, r, w, q='sync'):
        S.dma(lambda e: e.dma_start(out=out, in_=in_), bl(r), bl(w), q=q)

    PS = [Tl(top.enter_context(nc.psum_tensor("ps%d" % i, [128, 512], F32)), "ps%d" % i) for i in range(8)]

    def psb(i):
        return PS[i].t[:].bitcast(BF16)

    identF = sb(top, [128, 128], F32, "identF")
    identB = sb(top, [128, 128], BF16, "identB")
    onesF = sb(top, [128, 128], F32, "onesF")
    onesB = sb(top, [128, 128], BF16, "onesB")
    triU = sb(top, [128, 128], BF16, "triU")
    opad = [sb(top, [128, 128], BF16, "opad%d" % i) for i in range(2)]
    ones3 = sb(top, [3, 128], BF16, "ones3")
    gst = sb(top, [40, 128], F32, "gst")
    gT = sb(top, [128, 40], F32, "gT")
    invf = sb(top, [128, 8], F32, "invf")
    bfneg = sb(top, [8, 2], F32, "bfneg")
    pow2 = sb(top, [128, NBIS + 1], F32, "pow2")
    tauc = sb(top, [128, 1], F32, "tauc")
    rden = sb(top, [128, 512], F32, "rden")
    BIG = 30000.0
    cmask = sb(top, [128, 128], F32, "cmask")
    mset('pool', cmask[:], 0.0, [cmask])
    asel(cmask[:], cmask[:], [[-1, 128]], ALU.is_ge, NEG, 0, 1, [cmask], [cmask])
    maskS = [sb(top, [128, 512], BF16, "maskS%d" % j) for j in range(4)]
    posA = [sb(top, [128, 512], BF16, "posA%d" % j) for j in range(4)]
    negB = [sb(top, [128, 512], BF16, "negB%d" % j) for j in range(4)]
    for j in range(4):
        mset('pool', maskS[j][:], 1.0, [maskS[j]])
        asel(maskS[j][:], maskS[j][:], [[1, 512]], ALU.is_gt, 0.0, -128 * j, -1, [maskS[j]], [maskS[j]])
        mset('pool', posA[j][:], 0.0, [posA[j]])
        asel(posA[j][:], posA[j][:], [[1, 512]], ALU.is_gt, BIG, -128 * j, -1, [posA[j]], [posA[j]])
        mset('pool', negB[j][:], 0.0, [negB[j]])
        asel(negB[j][:], negB[j][:], [[1, 512]], ALU.is_ge, -BIG, -128 * j, -1, [negB[j]], [negB[j]])

    for t_, dtv in ((identF, 0.0), (identB, 0.0)):
        mset('pool', t_[:], 0.0, [t_])
        asel(t_[:], t_[:], [[-1, 128]], ALU.not_equal, 1.0, 0, 1, [t_], [t_])
    mset('pool', onesF[:], 1.0, [onesF])
    mset('pool', onesB[:], 1.0, [onesB])
    mset('pool', ones3[:], 1.0, [ones3])
    mset('pool', triU[:], 1.0, [triU])
    asel(triU[:], triU[:], [[-1, 128]], ALU.is_gt, 0.0, 0, 1, [triU], [triU])
    for i in range(2):
        mset('pool', opad[i][:], 0.0, [opad[i]])
        mset('pool', opad[i][:, i * 64:(i + 1) * 64], 1.0, [opad[i]])
    invfreq = (np.float32(500000.0) ** (-(np.arange(0, 16, 2, dtype=np.float32)) / np.float32(16))).astype(np.float32)
    for j in range(8):
        mset('pool', invf[:, j:j + 1], float(invfreq[j]), [invf])
    for k in range(NBIS + 1):
        mset('pool', pow2[:, k:k + 1], float(2.0 ** -(k + 1)), [pow2])
    mset('pool', tauc[:], -1.0e29, [tauc])
    for r_, src in ((0, gmix_d[0]), (1, gmix_d[DEPTH - 1]), (2, gmlp_d[0]), (3, gmlp_d[DEPTH - 1]), (4, gfin_d)):
        dma(gst[r_ * 8:(r_ + 1) * 8, :], src.rearrange("(c p) -> c p", p=128), [], [gst])
    tr(PS[0][:, 0:40], gst[0:40, :], identF[0:40, 0:40], [gst, identF], [PS[0]])
    cpy('dve', gT[:], PS[0][:, 0:40], [PS[0]], [gT])
    for l in range(DEPTH):
        dma(bfneg[:, l:l + 1], bf_d[l].rearrange("(h o) -> h o", o=1), [], [bfneg])
    ts('dve', bfneg[:, 0:DEPTH], bfneg[:, 0:DEPTH], -1.0, None, ALU.mult, None, [bfneg], [bfneg])

    wbuf = {}

    def cast2d(dst, src, rows, key):
        bs = []
        for r0 in range(0, rows, 128):
            b = Buf('%s_%d' % (str(key), r0))
            bs.append(b)
            dma(dst[r0:r0 + 128, :], src[r0:r0 + 128, :], [], [b], q='pool')
        wbuf[key] = bs

    def cast_layer(l):
        cast2d(winb[l], win_d[l], DM, ('in', l))
        cast2d(wbrb[l].rearrange("b k f -> (b k) f"), wbr_d[l].rearrange("b k f -> (b k) f"), 1536, ('br', l))
        cast2d(woutb[l], wout_d[l], DM, ('out', l))
        cast2d(wupb[l], wup_d[l], DM, ('up', l))
        cast2d(wdnb[l], wdn_d[l], 4 * DM, ('dn', l))

    cast_layer(0)
    cast_done = [1]

    def wload(tile, dst_ap, key, src2d, c0, c1):
        dma(dst_ap, src2d[:, c0:c1].rearrange("(kc p) f -> p kc f", p=128), wbuf[key], [tile])

    def norm_block(st_tmp, xblk, gidx, outs, out_bufs, ps_i, tmp):
        sq, rs, rs2 = tmp
        act(sq[:], xblk[:], AF.Square, [xblk], [sq])
        for c in range(KC):
            mm(PS[ps_i][:], onesF[:], sq[:, c, :], c == 0, c == KC - 1, [onesF, sq], [PS[ps_i]])
        ts('dve', rs[:], PS[ps_i][:], 1.0 / DM, 1e-6, ALU.mult, ALU.add, [PS[ps_i]], [rs])
        act(rs2[:], rs[:], AF.Sqrt, [rs], [rs2])
        S.op('dve', lambda e: e.reciprocal(out=rs[:], in_=rs2[:]), bl([rs2]), bl([rs]))
        for c in range(KC):
            stt('dve', outs(c), xblk[:, c, :], gT[:, gidx * 8 + c:gidx * 8 + c + 1], rs[:], ALU.mult, ALU.mult,
                [xblk, gT, rs], out_bufs)

    def run_chains(chains):
        chains = list(chains)
        while chains:
            nxt = []
            for c in chains:
                try:
                    next(c)
                    nxt.append(c)
                except StopIteration:
                    pass
            chains = nxt

    evac_rr = [0]

    def evac(out, in_, r, w):
        evac_rr[0] += 1
        if evac_rr[0] % 2:
            cpy('act', out, in_, r, w)
        else:
            cpy('dve', out, in_, r, w)

    slc = [0]

    def chk(k):
        if stop <= k + 10 * slc[0]:
            raise StopBuild()

    def body():
      for s in range(NSEQ):
        seqst = ExitStack()
        cosT = sb(seqst, [128, NT, 8], F32, "cosT")
        sinT = sb(seqst, [128, NT, 8], F32, "sinT")
        with ExitStack() as st:
            xtm = [sb(st, [128, DM], F32, "xtm") for _ in range(2)]
            xblk = [sb(st, [128, KC, 512], F32, "xblk") for _ in range(2)]
            xsb = Buf('xs')
            for tb in range(NB):
                xb_ = xblk[tb % 2]
                for t4 in range(4):
                    tt_ = tb * 4 + t4
                    xt = xtm[tt_ % 2]
                    dma(xt[:], x_d[s][tt_ * 128:(tt_ + 1) * 128, :], [], [xt])
                    for half in range(2):
                        pb = PS[half]
                        for c4 in range(4):
                            c = half * 4 + c4
                            tr(pb[:, c4 * 128:(c4 + 1) * 128], xt[:, c * 128:(c + 1) * 128], identF[:],
                               [xt, identF], [pb])
                        cpy('dve', xb_[:, half * 4:(half + 1) * 4, t4 * 128:(t4 + 1) * 128],
                            pb[:].rearrange("p (c t) -> p c t", t=128), [pb], [xb_])
                dma(xs_d[s][:, :, tb * 512:(tb + 1) * 512].rearrange("c p t -> p c t"), xb_[:], [xb_], [xsb])
            posi = sb(st, [16, 128], I32, "posi")
            posf = sb(st, [16, 128], F32, "posf")
            posT = sb(st, [128, 16], F32, "posT")
            ang = sb(st, [128, NT, 8], F32, "ang")
            kf = sb(st, [128, NT, 8], F32, "kf")
            ki_ = sb(st, [128, NT, 8], I32, "ki")
            dma(posi[:], pos_d[s].rearrange("(t p) -> t p", p=128), [], [posi])
            cpy('dve', posf[:], posi[:], [posi], [posf])
            tr(PS[2][:, 0:16], posf[0:16, :], identF[0:16, 0:16], [posf, identF], [PS[2]])
            cpy('dve', posT[:], PS[2][:, 0:16], [PS[2]], [posT])
            for tab, shift in ((sinT, 0.0), (cosT, PI / 2)):
                tt('dve', ang[:], posT[:].unsqueeze(2).to_broadcast([128, NT, 8]),
                   invf[:].unsqueeze(1).to_broadcast([128, NT, 8]), ALU.mult, [posT, invf], [ang])
                if shift:
                    ts('dve', ang[:], ang[:], shift, None, ALU.add, None, [ang], [ang])
                ts('dve', kf[:], ang[:], 1.0 / TWO_PI, None, ALU.mult, None, [ang], [kf])
                cpy('dve', ki_[:], kf[:], [kf], [ki_])
                cpy('dve', kf[:], ki_[:], [ki_], [kf])
                stt('dve', ang[:], kf[:], -TWO_PI, ang[:], ALU.mult, ALU.add, [kf, ang], [ang])
                ts('dve', kf[:], ang[:], PI, TWO_PI, ALU.is_gt, ALU.mult, [ang], [kf])
                tt('dve', ang[:], ang[:], kf[:], ALU.subtract, [ang, kf], [ang])
                ts('dve', kf[:], ang[:], -PI, TWO_PI, ALU.is_lt, ALU.mult, [ang], [kf])
                tt('dve', ang[:], ang[:], kf[:], ALU.add, [ang, kf], [ang])
                act(tab[:], ang[:], AF.Sin, [ang], [tab])
        S.fence()
        chk(1)

        for l in range(DEPTH):
            slc[0] = s * DEPTH + l
            last_layer = (l == DEPTH - 1)
            lay = ExitStack()
            hT = sb(lay, [128, KC, SL], BF16, "hT")
            with ExitStack() as st:
                xblk = [sb(st, [128, KC, 512], F32, "xblk") for _ in range(2)]
                sq = sb(st, [128, KC, 512], F32, "sq")
                rs = sb(st, [128, 512], F32, "rs")
                rs2 = sb(st, [128, 512], F32, "rs2")
                for tb in range(NB):
                    xb_ = xblk[tb % 2]
                    dma(xb_[:], xs_d[s][:, :, tb * 512:(tb + 1) * 512].rearrange("c p t -> p c t"), [xsb], [xb_])
                    norm_block(st, xb_, l, lambda c, tb=tb: hT[:, c, tb * 512:(tb + 1) * 512], [hT], 0, (sq, rs, rs2))
            S.fence()
            if dbg and s == 0 and l == 0:
                dma(dbg_h, hT[:], [hT], [], q='pool')
            chk(2)

            inner = ExitStack()
            brT = [sb(inner, [128, 4, SL], BF16, "brT%d" % b) for b in range(3)]

            def proj_fm(wt, col0, dst_fn, dst, pbanks):
                for tb in range(NB):
                    pb = PS[pbanks[tb % 2]]
                    for kc in range(KC):
                        mm(pb[:], wt[:, kc, col0:col0 + 128], hT[:, kc, tb * 512:(tb + 1) * 512], kc == 0, kc == KC - 1,
                           [wt, hT], [pb])
                    evac(dst_fn(tb), pb[:], [pb], [dst])

            def proj_vpad(wt, col0, vp, pbanks):
                for g4 in range(4):
                    pb = PS[pbanks[g4 % 2]]
                    for t4 in range(4):
                        tt_ = g4 * 4 + t4
                        for kc in range(KC):
                            mm(pb[:, t4 * 128:(t4 + 1) * 128], hT[:, kc, tt_ * 128:(tt_ + 1) * 128],
                               wt[:, kc, col0:col0 + 128], kc == 0, kc == KC - 1, [wt, hT], [pb])
                    pv = pb[:].rearrange("p (t f) -> p t f", f=128)
                    for hl in range(2):
                        cpy('dve', vp[hl][:, g4 * 4:(g4 + 1) * 4, hl * 64:(hl + 1) * 64], pv[:, :, hl * 64:(hl + 1) * 64],
                            [pb], [vp[hl]])

            with ExitStack() as st:
                wq = sb(st, [128, KC, 512], BF16, "wq"); wk = sb(st, [128, KC, 512], BF16, "wk")
                wv = sb(st, [128, KC, 512], BF16, "wv")
                for wt, c0 in ((wq, QA), (wk, KA), (wv, VA)):
                    wload(wt, wt[:], ('in', l), winb[l], c0, c0 + 512)
                qTp = [sb(st, [128, SL], BF16, "qTp") for _ in range(2)]
                kTp = [sb(st, [128, SL], BF16, "kTp") for _ in range(2)]
                vpd = [[sb(st, [128, NT, 128], BF16, "vpd") for _ in range(2)] for _ in range(2)]
                for a in vpd:
                    for v_ in a:
                        mset('pool', v_[:], 0.0, [v_])
                W = []
                for ch in range(2):
                    W.append(dict(e=sb(st, [128, 512], F32, "e"), sp=sb(st, [128, 512], F32, "sp"),
                                  nl=sb(st, [128, 512], BF16, "nl"), R=sb(st, [128, 512], F32, "R"),
                                  Rb=sb(st, [128, 512], BF16, "Rb"), tsum=sb(st, [128, 512], F32, "tsum"),
                                  wT=sb(st, [128, 512], BF16, "wT"), z=PS[2 + ch], lat=PS[4 + ch]))

                def chainA(hl, qT, kT, vp, Q, acc, w_):
                    base = hl * 64
                    nk = 4 * Q + 4
                    for idx, kt in enumerate(range(nk - 1, -1, -1)):
                        diag = kt >= 4 * Q
                        j = kt - 4 * Q
                        mm(w_['z'][:], kT[base:base + 64, kt * 128:(kt + 1) * 128],
                           qT[base:base + 64, Q * 512:(Q + 1) * 512], True, True, [kT, qT], [w_['z']])
                        yield
                        act(w_['e'][:], w_['z'][:], AF.Exp, [w_['z']], [w_['e']], scale=-SCALE)
                        yield
                        act(w_['sp'][:], w_['e'][:], AF.Ln, [w_['e']], [w_['sp']], bias=1.0)
                        yield
                        stt('dve', w_['nl'][:], w_['z'][:], SCALE, w_['sp'][:], ALU.mult, ALU.add,
                            [w_['z'], w_['sp']], [w_['nl']])
                        yield
                        if diag:
                            tt('pool', w_['nl'][:], w_['nl'][:], maskS[j][:], ALU.mult, [w_['nl'], maskS[j]], [w_['nl']])
                            yield
                        last_lat = (idx == 0) and not diag
                        mm(w_['lat'][:], triU[:], w_['nl'][:], True, (idx == 0 and not diag), [triU, w_['nl']], [w_['lat']])
                        if idx > 0:
                            mm(w_['lat'][:], onesB[:], w_['Rb'][:], False, not diag, [onesB, w_['Rb']], [w_['lat']])
                        if diag:
                            mm(w_['lat'][:], identB[:], posA[j][:], False, True, [identB, posA[j]], [w_['lat']])
                        yield
                        tt('dve', w_['tsum'][:], w_['lat'][:], w_['sp'][:], ALU.add, [w_['lat'], w_['sp']], [w_['tsum']])
                        yield
                        act(w_['wT'][:], w_['tsum'][:], AF.Exp, [w_['tsum']], [w_['wT']], scale=-1.0)
                        yield
                        mm(acc[:], vp[hl][:, kt, :], w_['wT'][:], hl == 0 and idx == 0, hl == 1 and idx == nk - 1,
                           [vp[hl], w_['wT']], [acc])
                        yield
                        if kt > 0:
                            if idx == 0:
                                cpy('pool', w_['R'][:], w_['nl'][:], [w_['nl']], [w_['R']])
                            else:
                                tt('pool', w_['R'][:], w_['R'][:], w_['nl'][:], ALU.add, [w_['R'], w_['nl']], [w_['R']])
                            yield
                            cpy('pool', w_['Rb'][:], w_['R'][:], [w_['R']], [w_['Rb']])
                            yield

                for hp in range(4):
                    i_ = hp % 2
                    chk(2.1)
                    proj_fm(wq, hp * 128, lambda tb: qTp[i_][:, tb * 512:(tb + 1) * 512], qTp[i_], (0, 1))
                    chk(2.15)
                    proj_fm(wk, hp * 128, lambda tb: kTp[i_][:, tb * 512:(tb + 1) * 512], kTp[i_], (0, 1))
                    chk(2.17)
                    proj_vpad(wv, hp * 128, vpd[i_], (0, 1))
                    chk(2.2)
                    for Q in range(NB):
                        acc = PS[6 + (Q % 2)]
                        run_chains([chainA(hl, qTp[i_], kTp[i_], vpd[i_], Q, acc, W[hl]) for hl in range(2)])
                        evac(brT[0][:, hp, Q * 512:(Q + 1) * 512], acc[:], [acc], [brT[0]])
                        chk(2.5)
            S.fence()
            if dbg and s == 0 and l == 0:
                dma(dbg_br[0], brT[0][:], [brT[0]], [], q='pool')
            chk(3)

            with ExitStack() as st:
                wq = sb(st, [128, KC, 512], BF16, "wq"); wk = sb(st, [128, KC, 512], BF16, "wk")
                wv = sb(st, [128, KC, 512], BF16, "wv")
                wfl = sb(st, [128, KC, 8], BF16, "wfl")
                for wt, c0 in ((wq, QB), (wk, KB), (wv, VB)):
                    wload(wt, wt[:], ('in', l), winb[l], c0, c0 + 512)
                wload(wfl, wfl[:], ('in', l), winb[l], FLC, FLC + 8)
                qTp = [sb(st, [128, SL], BF16, "qTp") for _ in range(2)]
                kTp = [sb(st, [128, SL], BF16, "kTp") for _ in range(2)]
                vpd = [[sb(st, [128, NT, 128], BF16, "vpd") for _ in range(2)] for _ in range(2)]
                for a in vpd:
                    for v_ in a:
                        mset('pool', v_[:], 0.0, [v_])
                augp = [sb(st, [3, 2, SL], BF16, "augp") for _ in range(2)]
                cnegT = sb(st, [128, NT, 8], F32, "cnegT")
                augb = Buf('augd')
                with ExitStack() as st2:
                    lfe = sb(st2, [8, SL], F32, "lfe")
                    cneg = sb(st2, [8, SL], F32, "cneg"); v8 = sb(st2, [8, SL], F32, "v8")
                    part = sb(st2, [8, SL], BF16, "part")
                    for tb in range(NB):
                        pb = PS[tb % 2]
                        for kc in range(KC):
                            mm(pb[0:8, :], wfl[:, kc, :], hT[:, kc, tb * 512:(tb + 1) * 512], kc == 0, kc == KC - 1,
                               [wfl, hT], [pb])
                        act(lfe[:, tb * 512:(tb + 1) * 512], pb[0:8, :], AF.Exp, [pb, bfneg], [lfe],
                            bias=bfneg[:, l:l + 1], scale=-1.0)
                    act(lfe[:], lfe[:], AF.Ln, [lfe], [lfe], bias=1.0)
                    S.op('dve', lambda e: e.tensor_tensor_scan(out=cneg[:], data0=lfe[:], data1=lfe[:], initial=0.0,
                                                               op0=ALU.add, op1=ALU.bypass), bl([lfe]), bl([cneg]))
                    ts('dve', v8[:], cneg[:], -8.0, None, ALU.mult, None, [cneg], [v8])
                    for pi_ in range(3):
                        cpy('dve', part[:], v8[:], [v8], [part])
                        if pi_ < 2:
                            tt('dve', v8[:], v8[:], part[:], ALU.subtract, [v8, part], [v8])
                        dma(aug_d[pi_], part[:], [part], [augb])
                    for tt_ in range(NT):
                        tr(PS[2][:, tt_ * 8:(tt_ + 1) * 8], cneg[0:8, tt_ * 128:(tt_ + 1) * 128], identF[0:8, 0:8],
                           [cneg, identF], [PS[2]])
                    cpy('dve', cnegT[:], PS[2][:, 0:128].rearrange("p (t h) -> p t h", h=8), [PS[2]], [cnegT])
                S.fence()
                W = []
                for ch in range(2):
                    W.append(dict(pT=sb(st, [128, 512], BF16, "pT"), z=PS[2 + ch]))

                def chainB(hl, h, qT, kT, vp, aq, Q, num, den, w_):
                    base = hl * 64
                    nk = 4 * Q + 4
                    for idx, kt in enumerate(range(nk)):
                        diag = kt >= 4 * Q
                        j = kt - 4 * Q
                        mm(w_['z'][:], kT[base:base + 64, kt * 128:(kt + 1) * 128],
                           qT[base:base + 64, Q * 512:(Q + 1) * 512], True, False, [kT, qT], [w_['z']])
                        mm(w_['z'][:], ones3[0:3, :], aq[0:3, hl, Q * 512:(Q + 1) * 512], False, not diag,
                           [ones3, aq], [w_['z']])
                        if diag:
                            mm(w_['z'][:], identB[:], negB[j][:], False, True, [identB, negB[j]], [w_['z']])
                        yield
                        act(w_['pT'][:], w_['z'][:], AF.Exp, [w_['z'], cnegT], [w_['pT']],
                            bias=cnegT[:, kt, h:h + 1], scale=SCALE)
                        yield
                        first = (hl == 0 and idx == 0)
                        lastf = (hl == 1 and idx == nk - 1)
                        mm(num[:], vp[hl][:, kt, :], w_['pT'][:], first, lastf, [vp[hl], w_['pT']], [num])
                        mm(den[:], opad[hl][:], w_['pT'][:], first, lastf, [opad[hl], w_['pT']], [den])
                        yield

                for hp in range(4):
                    i_ = hp % 2
                    proj_fm(wq, hp * 128, lambda tb: qTp[i_][:, tb * 512:(tb + 1) * 512], qTp[i_], (0, 1))
                    proj_fm(wk, hp * 128, lambda tb: kTp[i_][:, tb * 512:(tb + 1) * 512], kTp[i_], (0, 1))
                    proj_vpad(wv, hp * 128, vpd[i_], (0, 1))
                    dma(augp[i_][:], aug_d[:, 2 * hp:2 * hp + 2, :], [augb], [augp[i_]])
                    for Q in range(NB):
                        num = PS[4 + 2 * (Q % 2)]
                        den = PS[5 + 2 * (Q % 2)]
                        run_chains([chainB(hl, 2 * hp + hl, qTp[i_], kTp[i_], vpd[i_], augp[i_], Q, num, den, W[hl])
                                    for hl in range(2)])
                        S.op('dve', lambda e, den=den: e.reciprocal(out=rden[:], in_=den[:]), bl([den]), bl([rden]))
                        tt('dve', brT[1][:, hp, Q * 512:(Q + 1) * 512], num[:], rden[:], ALU.mult, [num, rden], [brT[1]])
            S.fence()
            if dbg and s == 0 and l == 0:
                dma(dbg_br[1], brT[1][:], [brT[1]], [], q='pool')
            chk(4)

            with ExitStack() as st:
                qcT = sb(st, [128, 4, SL], BF16, "qcT")
                kcTd = sb(st, [128, 2, SL], BF16, "kcTd")
                qiT = sb(st, [128, 2, SL], BF16, "qiT")
                kiT2 = sb(st, [128, SL], BF16, "kiT2")
                vpc = [[sb(st, [128, NT, 128], BF16, "vpc") for _ in range(2)] for _ in range(2)]
                wiT = sb(st, [128, NT, 4], F32, "wiT")
                for a in vpc:
                    for v_ in a:
                        mset('pool', v_[:], 0.0, [v_])
                with ExitStack() as st2:
                    wc = sb(st2, [128, KC, CW], BF16, "wc")
                    wload(wc, wc[:], ('in', l), winb[l], CC, CC + CW)
                    cq = sb(st2, [128, 512], F32, "cq")
                    c1 = sb(st2, [128, 512], F32, "c1")
                    c2 = sb(st2, [128, 128], F32, "c2")
                    kd = sb(st2, [128, 128], F32, "kd")
                    kcd = sb(st2, [128, 2, 128], F32, "kcd")
                    rt = [sb(st2, [128, 8, 8], F32, "rt") for _ in range(4)]

                    def rope(t_, ncol, nh, tt_):
                        dv = t_[:, 0:ncol].rearrange("p (h d) -> p h d", d=64)
                        cs = cosT[:, tt_, :].unsqueeze(1).to_broadcast([128, nh, 8])
                        sn = sinT[:, tt_, :].unsqueeze(1).to_broadcast([128, nh, 8])
                        x1 = dv[:, :, 0:8]
                        x2 = dv[:, :, 8:16]
                        r0, r1, r2, r3 = [r_[:, 0:nh, :] for r_ in rt]
                        tt('dve', r0, x1, cs, ALU.mult, [t_, cosT], [rt[0]])
                        tt('dve', r1, x2, sn, ALU.mult, [t_, sinT], [rt[1]])
                        tt('dve', r2, x2, cs, ALU.mult, [t_, cosT], [rt[2]])
                        tt('dve', r3, x1, sn, ALU.mult, [t_, sinT], [rt[3]])
                        tt('dve', x1, r0, r1, ALU.subtract, [rt[0], rt[1], t_], [t_])
                        tt('dve', x2, r2, r3, ALU.add, [rt[2], rt[3], t_], [t_])

                    for tt_ in range(NT):
                        tsl = slice(tt_ * 128, (tt_ + 1) * 128)
                        for bi, (c0, cw_) in enumerate(((0, 512), (512, 512), (1024, 68))):
                            pb = PS[bi]
                            for kc in range(KC):
                                mm(pb[:, 0:cw_], hT[:, kc, tsl], wc[:, kc, c0:c0 + cw_], kc == 0, kc == KC - 1,
                                   [wc, hT], [pb])
                        cpy('act', cq[:], PS[0][:], [PS[0]], [cq])
                        cpy('act', c1[:], PS[1][:], [PS[1]], [c1])
                        cpy('dve', c2[:, 0:68], PS[2][:, 0:68], [PS[2]], [c2])
                        rope(cq, 512, 8, tt_)
                        rope(c1, 512, 8, tt_)
                        rope(c2, 64, 1, tt_)
                        cpy('pool', kd[:, 0:64], c2[:, 0:64], [c2], [kd])
                        cpy('pool', kd[:, 64:128], c2[:, 0:64], [c2], [kd])
                        ts('dve', wiT[:, tt_, :], c2[:, 64:68], 0.5, None, ALU.mult, None, [c2], [wiT])
                        for g in range(2):
                            for half in range(2):
                                cpy('dve', vpc[g][half][:, tt_, half * 64:(half + 1) * 64],
                                    PS[1][:, 128 + g * 64:128 + (g + 1) * 64], [PS[1]], [vpc[g][half]])
                                cpy('pool', kcd[:, g, half * 64:(half + 1) * 64], c1[:, g * 64:(g + 1) * 64],
                                    [c1], [kcd])
                        for c in range(4):
                            tr(PS[3][:, c * 128:(c + 1) * 128], cq[:, c * 128:(c + 1) * 128], identF[:],
                               [cq, identF], [PS[3]])
                        cpy('dve', qcT[:, :, tsl], PS[3][:].rearrange("p (c t) -> p c t", t=128), [PS[3]], [qcT])
                        for g in range(2):
                            tr(PS[4][:, g * 128:(g + 1) * 128], kcd[:, g, :], identF[:], [kcd, identF], [PS[4]])
                        for c in range(2):
                            tr(PS[4][:, (2 + c) * 128:(3 + c) * 128], c1[:, 256 + c * 128:256 + (c + 1) * 128], identF[:],
                               [c1, identF], [PS[4]])
                        tr(PS[5][:, 0:128], kd[:], identF[:], [kd, identF], [PS[5]])
                        cpy('dve', kcTd[:, :, tsl], PS[4][:, 0:256].rearrange("p (c t) -> p c t", t=128), [PS[4]], [kcTd])
                        cpy('dve', qiT[:, :, tsl], PS[4][:, 256:512].rearrange("p (c t) -> p c t", t=128), [PS[4]], [qiT])
                        cpy('dve', kiT2[:, tsl], PS[5][:, 0:128], [PS[5]], [kiT2])
                S.fence()
                chk(4.3)
                with ExitStack() as st3:
                    sc = [sb(st3, [128, SL], F32, "sc") for _ in range(2)]
                    junk2 = [sb(st3, [128, SL], BF16, "junk") for _ in range(2)]
                    mk = [sb(st3, [128, SL], F32, "mk") for _ in range(1)]
                    mkT = [sb(st3, [128, NT, 512], BF16, "mkT") for _ in range(1)]
                    rl = [sb(st3, [128, 512], F32, "rl") for _ in range(2)]
                    BST = [dict(mx=sb(st3, [128, 1], F32, "mx"), mn=sb(st3, [128, 1], F32, "mn"),
                                wd=sb(st3, [128, 1], F32, "wd"), hs=sb(st3, [128, NBIS + 1], F32, "hs"),
                                mids=sb(st3, [128, NBIS + 1], F32, "mids"), cnts=sb(st3, [128, NBIS], F32, "cnts"),
                                gg=sb(st3, [128, 1], F32, "gg")) for _ in range(2)]
                    W = []
                    for ch in range(2):
                        W.append(dict(pT=sb(st3, [128, 512], BF16, "pT"), z=PS[4 + ch]))
                    rlc = [0]

                    def indexer(i):
                        Q = i // 4
                        j = i % 4
                        n = (i + 1) * 128
                        nfull = (4 * Q + 4) * 128
                        sc_ = sc[i % 2]
                        b_ = BST[i % 2]
                        mx, mn, wd, hs, mids, cnts, gg = (b_['mx'], b_['mn'], b_['wd'], b_['hs'], b_['mids'],
                                                          b_['cnts'], b_['gg'])
                        junk = junk2[i % 2]
                        mk_ = mk[0]
                        qsl = slice(i * 128, (i + 1) * 128)
                        for c0 in range(0, n, 512):
                            cw_ = min(512, n - c0)
                            for h in range(4):
                                pb = PS[rlc[0] % 2]
                                r_ = rl[rlc[0] % 2]
                                rlc[0] += 1
                                hb = (h % 2) * 64
                                mm(pb[:, 0:cw_], qiT[hb:hb + 64, h // 2, qsl], kiT2[hb:hb + 64, c0:c0 + cw_], True, True,
                                   [qiT, kiT2], [pb])
                                act(r_[:, 0:cw_], pb[:, 0:cw_], AF.Relu, [pb], [r_], scale=0.125)
                                if h == 0:
                                    ts('dve', sc_[:, c0:c0 + cw_], r_[:, 0:cw_], wiT[:, i, 0:1], None, ALU.mult, None,
                                       [r_, wiT], [sc_])
                                else:
                                    stt('dve', sc_[:, c0:c0 + cw_], r_[:, 0:cw_], wiT[:, i, h:h + 1], sc_[:, c0:c0 + cw_],
                                        ALU.mult, ALU.add, [r_, wiT, sc_], [sc_])
                                yield
                        if i >= 2:
                            S.op('dve', lambda e: e.tensor_reduce(out=mx[:], in_=sc_[:, 0:n], axis=AX.X, op=ALU.max),
                                 bl([sc_]), bl([mx]))
                            S.op('dve', lambda e: e.tensor_reduce(out=mn[:], in_=sc_[:, 0:n], axis=AX.X, op=ALU.min),
                                 bl([sc_]), bl([mn]))
                        tt('pool', sc_[:, qsl], sc_[:, qsl], cmask[:], ALU.add, [sc_, cmask], [sc_])
                        if n < nfull:
                            mset('pool', sc_[:, n:nfull], NEG, [sc_])
                        if i >= 2:
                            stt('dve', wd[:], mx[:], 1.0, mn[:], ALU.add, ALU.subtract, [mx, mn], [wd])
                            ts('dve', hs[:], pow2[:], wd[:, 0:1], None, ALU.mult, None, [pow2, wd], [hs])
                            tt('dve', mids[:, 0:1], mn[:], hs[:, 0:1], ALU.add, [mn, hs], [mids])
                            mset('dve', cnts[:], 0.0, [cnts])
                            yield
                            for k in range(NBIS):
                                ts('dve', junk[:, 0:n], sc_[:, 0:n], mids[:, k:k + 1], 0.0, ALU.is_ge, ALU.add,
                                   [sc_, mids], [junk, cnts], accum=cnts[:, k:k + 1])
                                yield
                                si = k + 1 if k < NBIS - 1 else k
                                ts('dve', gg[:], cnts[:, k:k + 1], 255.5, hs[:, k:k + 1], ALU.is_gt, ALU.mult,
                                   [cnts, hs], [gg])
                                yield
                                stt('dve', mids[:, k + 1:k + 2], gg[:], hs[:, si:si + 1], mids[:, k:k + 1],
                                    ALU.subtract, ALU.add, [gg, hs, mids], [mids])
                                yield
                            tau = mids[:, NBIS:NBIS + 1]
                            taub = mids
                        else:
                            tau = tauc[:, 0:1]
                            taub = tauc
                        ts('dve', mk_[:, 0:nfull], sc_[:, 0:nfull], tau, None, ALU.is_ge, None, [sc_, taub], [mk_])
                        mt = mkT[0]
                        nkt = 4 * Q + 4
                        for k0 in range(0, nkt, 4):
                            kn = min(4, nkt - k0)
                            pbi = 2 + ((k0 // 4) % 2)
                            for kk in range(kn):
                                kt = k0 + kk
                                tr(PS[pbi][:, kk * 128:(kk + 1) * 128], mk_[:, kt * 128:(kt + 1) * 128], identF[:],
                                   [mk_, identF], [PS[pbi]])
                            cpy('dve', mt[:, k0:k0 + kn, j * 128:(j + 1) * 128],
                                PS[pbi][:, 0:kn * 128].rearrange("p (k t) -> p k t", t=128), [PS[pbi]], [mt])

                    def chainC(head, Q, num, den, w_, first_chain, last_chain):
                        g = head // 4
                        half = head % 2
                        chunk = head // 2
                        base = half * 64
                        nk = 4 * Q + 4
                        mt = mkT[0]
                        for idx, kt in enumerate(range(nk)):
                            mm(w_['z'][:], kcTd[base:base + 64, g, kt * 128:(kt + 1) * 128],
                               qcT[base:base + 64, chunk, Q * 512:(Q + 1) * 512], True, True, [kcTd, qcT], [w_['z']])
                            yield
                            act(w_['pT'][:], w_['z'][:], AF.Exp, [w_['z']], [w_['pT']], scale=SCALE)
                            yield
                            tt('dve', w_['pT'][:], w_['pT'][:], mt[:, kt, :], ALU.mult, [w_['pT'], mt], [w_['pT']])
                            yield
                            first = first_chain and idx == 0
                            lastf = last_chain and idx == nk - 1
                            mm(num[:], vpc[g][half][:, kt, :], w_['pT'][:], first, lastf, [vpc[g][half], w_['pT']], [num])
                            mm(den[:], opad[half][:], w_['pT'][:], first, lastf, [opad[half], w_['pT']], [den])
                            yield

                    for Q in range(NB):
                        for j in range(0, 4, 2):
                            run_chains([indexer(4 * Q + j), indexer(4 * Q + j + 1)])
                            chk(4.4 if j < 2 else 4.5)
                        chk(4.6)
                        for hp in range(4):
                            num = PS[6]
                            den = PS[7]
                            run_chains([chainC(2 * hp + hl, Q, num, den, W[hl], hl == 0, hl == 1) for hl in range(2)])
                            S.op('dve', lambda e, den=den: e.reciprocal(out=rden[:], in_=den[:]), bl([den]), bl([rden]))
                            tt('dve', brT[2][:, hp, Q * 512:(Q + 1) * 512], num[:], rden[:], ALU.mult, [num, rden], [brT[2]])
                            chk(4.7)
            S.fence()
            if dbg and s == 0 and l == 0:
                dma(dbg_br[2], brT[2][:], [brT[2]], [], q='pool')
            chk(5)

            with ExitStack() as st:
                yT = sb(st, [128, KC, SL], BF16, "yT")
                wb_ = [sb(st, [128, 4, 512], BF16, "wb") for _ in range(3)]
                wg_ = [sb(st, [128, KC, 512], BF16, "wg") for _ in range(3)]
                sg = [sb(st, [128, 512], F32, "sg") for _ in range(2)]
                accy = sb(st, [128, 512], F32, "accy")
                tmpy = sb(st, [128, 512], F32, "tmpy")
                pc_ = [0]
                for fg in range(2):
                    for b in range(3):
                        dma(wb_[b][:], wbrb[l][b][:, fg * 512:(fg + 1) * 512].rearrange("(kc p) f -> p kc f", p=128),
                            wbuf[('br', l)], [wb_[b]])
                        wload(wg_[b], wg_[b][:], ('in', l), winb[l], GATE + b * DM + fg * 512, GATE + b * DM + (fg + 1) * 512)
                    for tb in range(NB):
                        tsl = slice(tb * 512, (tb + 1) * 512)
                        for fc in range(4):
                            c = fg * 4 + fc
                            fsl = slice(fc * 128, (fc + 1) * 128)
                            for b in range(3):
                                pp = PS[(pc_[0] % 4) * 2]
                                pg = PS[(pc_[0] % 4) * 2 + 1]
                                sg_ = sg[pc_[0] % 2]
                                pc_[0] += 1
                                for kc in range(KC):
                                    mm(pg[:], wg_[b][:, kc, fsl], hT[:, kc, tsl], kc == 0, kc == KC - 1, [wg_[b], hT], [pg])
                                for k4 in range(4):
                                    mm(pp[:], wb_[b][:, k4, fsl], brT[b][:, k4, tsl], k4 == 0, k4 == 3, [wb_[b], brT[b]], [pp])
                                act(sg_[:], pg[:], AF.Sigmoid, [pg], [sg_])
                                if b == 0:
                                    tt('dve', accy[:], pp[:], sg_[:], ALU.mult, [pp, sg_], [accy])
                                elif b == 1:
                                    tt('dve', tmpy[:], pp[:], sg_[:], ALU.mult, [pp, sg_], [tmpy])
                                    tt('pool', accy[:], accy[:], tmpy[:], ALU.add, [accy, tmpy], [accy])
                                else:
                                    tt('dve', tmpy[:], pp[:], sg_[:], ALU.mult, [pp, sg_], [tmpy])
                                    tt('pool', yT[:, c, tsl], accy[:], tmpy[:], ALU.add, [accy, tmpy], [yT])
                S.fence()
                for c in range(KC):
                    cpy('pool' if c % 2 else 'dve', hT[:, c, :], yT[:, c, :], [yT], [hT])
            S.fence()
            inner.close()
            yTT = hT
            chk(6)

            if cast_done[0] < DEPTH and cast_done[0] == l + 1:
                cast_layer(cast_done[0])
                cast_done[0] += 1
            with ExitStack() as st:
                wo = sb(st, [128, KC, DM], BF16, "wo")
                wload(wo, wo[:, :, 0:512], ('out', l), woutb[l], 0, 512)
                wload(wo, wo[:, :, 512:1024], ('out', l), woutb[l], 512, 1024)
                xblk = [sb(st, [128, KC, 512], F32, "xblk") for _ in range(1)]
                sq = sb(st, [128, KC, 512], F32, "sq")
                rs = sb(st, [128, 512], F32, "rs")
                rs2 = sb(st, [128, 512], F32, "rs2")
                h2T = sb(st, [128, KC, 512], BF16, "h2T")
                uT = sb(st, [128, 32, 512], BF16, "uT")
                wu = [sb(st, [128, KC, 512], BF16, "wu") for _ in range(2)]
                wd_ = [sb(st, [128, 16, 256], BF16, "wd") for _ in range(2)]
                wdc = [0]
                rl = [sb(st, [128, 512], F32, "rl") for _ in range(2)]
                otm = [sb(st, [128, DM], F32, "otm") for _ in range(2)] if last_layer else None
                fin = sb(st, [128, KC, 512], F32, "fin") if last_layer else None
                pcn = [0]
                xsb2 = Buf('xs2')
                for tb in range(NB):
                    tsl = slice(tb * 512, (tb + 1) * 512)
                    xb_ = xblk[0]
                    dma(xb_[:], xs_d[s][:, :, tsl].rearrange("c p t -> p c t"), [xsb], [xb_])
                    for fc in range(KC):
                        pb = PS[pcn[0] % 2]
                        pcn[0] += 1
                        for kc in range(KC):
                            mm(pb[:], wo[:, kc, fc * 128:(fc + 1) * 128], yTT[:, kc, tsl], kc == 0, kc == KC - 1,
                               [wo, yTT], [pb])
                        tt('dve', xb_[:, fc, :], pb[:], xb_[:, fc, :], ALU.add, [pb, xb_], [xb_])
                    norm_block(st, xb_, 2 + l, lambda c: h2T[:, c, :], [h2T], 2, (sq, rs, rs2))
                    for fg in range(8):
                        wu_ = wu[fg % 2]
                        wload(wu_, wu_[:], ('up', l), wupb[l], fg * 512, (fg + 1) * 512)
                        for fc in range(4):
                            pb = PS[pcn[0] % 2]
                            r_ = rl[pcn[0] % 2]
                            pcn[0] += 1
                            for kc in range(KC):
                                mm(pb[:], wu_[:, kc, fc * 128:(fc + 1) * 128], h2T[:, kc, :], kc == 0, kc == KC - 1,
                                   [wu_, h2T], [pb])
                            act(r_[:], pb[:], AF.Relu, [pb], [r_])
                            tt('dve', uT[:, fg * 4 + fc, :], r_[:], r_[:], ALU.mult, [r_], [uT])
                    for f2 in range(4):
                        pbs = [PS[3 + (f2 % 2) * 2 + fc] for fc in range(2)]
                        for hk in range(2):
                            wdt = wd_[wdc[0] % 2]
                            wdc[0] += 1
                            dma(wdt[:], wdnb[l][hk * 2048:(hk + 1) * 2048, f2 * 256:(f2 + 1) * 256].rearrange(
                                "(kc p) f -> p kc f", p=128), wbuf[('dn', l)], [wdt])
                            for fc in range(2):
                                for kc in range(16):
                                    mm(pbs[fc][:], wdt[:, kc, fc * 128:(fc + 1) * 128], uT[:, hk * 16 + kc, :],
                                       hk == 0 and kc == 0, hk == 1 and kc == 15, [wdt, uT], [pbs[fc]])
                        for fc in range(2):
                            c = f2 * 2 + fc
                            tt('dve', xb_[:, c, :], pbs[fc][:], xb_[:, c, :], ALU.add, [pbs[fc], xb_], [xb_])
                    if dbg and s == 0 and l == 0:
                        dma(dbg_x[:, :, tsl].rearrange("c p t -> p c t"), xb_[:], [xb_], [])
                    if not last_layer:
                        dma(xs_d[s][:, :, tsl].rearrange("c p t -> p c t"), xb_[:], [xb_], [xsb2])
                    else:
                        if final:
                            norm_block(st, xb_, 4, lambda c: fin[:, c, :], [fin], 2, (sq, rs, rs2))
                        else:
                            for c in range(KC):
                                cpy('pool' if c % 2 else 'dve', fin[:, c, :], xb_[:, c, :], [xb_], [fin])
                        for t4 in range(4):
                            ot = otm[t4 % 2]
                            for half in range(2):
                                pb = PS[5 + half]
                                for c4 in range(4):
                                    c = half * 4 + c4
                                    tr(pb[:, c4 * 128:(c4 + 1) * 128], fin[:, c, t4 * 128:(t4 + 1) * 128], identF[:],
                                       [fin, identF], [pb])
                                evac(ot[:, half * 512:(half + 1) * 512], pb[:], [pb], [ot])
                            tt_ = tb * 4 + t4
                            dma(out_d[s][tt_ * 128:(tt_ + 1) * 128, :], ot[:], [ot], [])
                xsb = xsb2
            S.fence()
            lay.close()
        seqst.close()
        S.fence()

    try:
        body()
    except StopBuild:
        S.fence()
    S.emit(top)
    top.close()
    return nc, S


_CACHE = {}


def kernel(x, positions, g_mix, w_in, b_forget, w_branch, w_out, g_mlp, w_up, w_down, g_final):
    n = 8
    if 'nc' not in _CACHE:
        _CACHE['nc'] = build(2, 2)[0]
    nc = _CACHE['nc']
    f32 = lambda a: np.ascontiguousarray(np.asarray(a, dtype=np.float32))
    x = f32(x)
    pos = np.ascontiguousarray(np.asarray(positions, dtype=np.int32))
    shared = dict(g_mix=f32(g_mix), w_in=f32(w_in), b_forget=f32(b_forget), w_branch=f32(w_branch),
                  w_out=f32(w_out), g_mlp=f32(g_mlp), w_up=f32(w_up), w_down=f32(w_down), g_final=f32(g_final))
    in_maps = []
    for c in range(n):
        m = dict(shared)
        m['x'] = np.ascontiguousarray(x[2 * c:2 * c + 2])
        m['positions'] = np.ascontiguousarray(pos[2 * c:2 * c + 2])
        in_maps.append(m)
    res = run_bass_kernel_spmd(nc, in_maps, core_ids=list(range(n)))
    return np.concatenate([np.asarray(r["out"], dtype=np.float32) for r in res.results], axis=0)
```

```python
import numpy as np
from contextlib import ExitStack
import concourse.bass as bass
import concourse.mybir as mybir
from concourse.bass_utils import run_bass_kernel_spmd

F32 = mybir.dt.float32
BF16 = mybir.dt.bfloat16
I32 = mybir.dt.int32
U32 = mybir.dt.uint32
AF = mybir.ActivationFunctionType
ALU = mybir.AluOpType
AX = mybir.AxisListType

CE = ('pe', 'act', 'dve', 'pool')
NCE = len(CE)
EPOCH = 4000
NDSEM = 12


class Buf:
    __slots__ = ('name', 'w', 'rs')

    def __init__(self, name=''):
        self.name = name
        self.w = None
        self.rs = []


class Op:
    __slots__ = ('eng', 'q', 'fn', 'idx', 'deps', 'waits', 'marked', 'semi', 'semv', 'clock', 'isdma', 'dnum')


class Sch:
    def __init__(self, nc):
        self.nc = nc
        self.ops = []
        self.cnt = {e: 0 for e in CE}
        self.last = {e: None for e in CE}
        self.ndma = {'sync': 0, 'pool': 0, 'act': 0}
        self.dmas = {'sync': [], 'pool': [], 'act': []}

    def _rec(self, o, reads, writes):
        deps = {}
        for b in reads:
            if b.w is not None:
                deps[id(b.w)] = b.w
        for b in writes:
            if b.w is not None:
                deps[id(b.w)] = b.w
            for r in b.rs:
                deps[id(r)] = r
        deps.pop(id(o), None)
        o.deps = list(deps.values())
        for b in reads:
            b.rs.append(o)
        for b in writes:
            b.w = o
            b.rs = []
        self.ops.append(o)

    def op(self, eng, fn, reads=(), writes=()):
        o = Op()
        o.eng = eng; o.q = eng; o.fn = fn; o.isdma = False
        self.cnt[eng] += 1
        o.idx = self.cnt[eng]
        o.marked = False
        self._rec(o, reads, writes)
        self.last[eng] = o
        return o

    def dma(self, fn, reads=(), writes=(), q='sync'):
        o = Op()
        o.eng = 'dma'; o.q = q; o.fn = fn; o.isdma = True
        o.dnum = self.ndma[q]
        self.ndma[q] += 1
        self.dmas[q].append(o)
        o.idx = 0
        o.marked = True
        self._rec(o, reads, writes)
        return o

    def fence(self):
        lst = [self.last[e] for e in CE if self.last[e] is not None]
        dl = []
        for q in self.dmas:
            dl += self.dmas[q][-NDSEM:]
        for e in CE + ('sync',):
            o = Op()
            o.eng = e; o.q = e; o.fn = None; o.isdma = False
            o.idx = 0
            o.marked = False
            o.deps = [d for d in lst if d.eng != e] + list(dl)
            self.ops.append(o)

    def plan(self):
        ei = {e: i for i, e in enumerate(CE)}
        run = {q: [0] * NCE for q in ('pe', 'act', 'dve', 'pool', 'sync')}
        seen_dma = {q: set() for q in run}
        for o in self.ops:
            q = o.q
            rc = run[q]
            waits = []
            for d in o.deps:
                if d.isdma:
                    if id(d) in seen_dma[q]:
                        continue
                    seen_dma[q].add(id(d))
                    waits.append(d)
                    dc = d.clock
                    for i in range(NCE):
                        if dc[i] > rc[i]:
                            rc[i] = dc[i]
                else:
                    if d.eng == 'pe' and o.eng == 'pe':
                        continue
                    j = ei[d.eng]
                    if rc[j] >= d.idx:
                        continue
                    waits.append(d)
                    d.marked = True
                    dc = d.clock
                    for i in range(NCE):
                        if dc[i] > rc[i]:
                            rc[i] = dc[i]
            best = {}
            fin = []
            for d in waits:
                if d.isdma:
                    fin.append(d)
                elif d.eng not in best or best[d.eng].idx < d.idx:
                    best[d.eng] = d
            fin.extend(best.values())
            o.waits = fin
            o.clock = list(rc)
            if not o.isdma and o.fn is not None:
                o.clock[ei[o.eng]] = o.idx

    def emit(self, stack):
        nc = self.nc
        self.plan()
        sems = {}

        def getsem(name):
            if name not in sems:
                sems[name] = stack.enter_context(nc.semaphore(name))
            return sems[name]

        mcount = {e: 0 for e in CE}
        for o in self.ops:
            if o.isdma:
                k = o.dnum % NDSEM
                o.semi = 'd_%s_%d' % (o.q, k)
                o.semv = 16 * (o.dnum // NDSEM + 1)
            elif o.marked:
                mcount[o.eng] += 1
                ep = (mcount[o.eng] - 1) // EPOCH
                o.semi = 'c_%s_%d' % (o.eng, ep)
                o.semv = mcount[o.eng] - ep * EPOCH
        for o in self.ops:
            if o.isdma or o.marked:
                getsem(o.semi)
        byq = {q: [] for q in ('pe', 'act', 'dve', 'pool', 'sync')}
        for o in self.ops:
            byq[o.q].append(o)
        self.nwaits = 0

        def run_queue(eng, lst):
            ring = {}
            for o in lst:
                for d in o.waits:
                    eng.wait_ge(sems[d.semi], d.semv)
                    self.nwaits += 1
                if o.isdma:
                    k = o.dnum % NDSEM
                    if k in ring:
                        p = ring[k]
                        eng.wait_ge(sems[p.semi], p.semv)
                    ring[k] = o
                    o.fn(eng).then_inc(sems[o.semi], 16)
                elif o.fn is not None:
                    ins = o.fn(eng)
                    if o.marked:
                        ins.then_inc(sems[o.semi], 1)

        with nc.Block() as block:
            @block.sync
            def _(e):
                run_queue(e, byq['sync'])
                for q in ('sync', 'pool', 'act'):
                    for o in self.dmas[q][-NDSEM:]:
                        e.wait_ge(sems[o.semi], o.semv)

            @block.tensor
            def _(e):
                run_queue(e, byq['pe'])

            @block.scalar
            def _(e):
                run_queue(e, byq['act'])

            @block.vector
            def _(e):
                run_queue(e, byq['dve'])

            @block.gpsimd
            def _(e):
                run_queue(e, byq['pool'])


SL = 2048
DM = 1024
NT = 16
NB = 4
KC = 8
QA, KA, VA, QB, KB, VB, FLC, CC, GATE = 0, 512, 1024, 1536, 2048, 2560, 3072, 3080, 4172
NCOL = 7244
CW = 1092
SCALE = 0.125
NEG = -1.0e30
NBIS = 22
TWO_PI = float(2 * np.pi)
PI = float(np.pi)


class Tl:
    def __init__(self, t, name=''):
        self.t = t
        self.b = Buf(name)

    def __getitem__(self, k):
        return self.t[k]


class StopBuild(Exception):
    pass


def build(NSEQ=2, DEPTH=2, dbg=False, stop=99, final=True):
    return _build(NSEQ, DEPTH, dbg, stop, final)


def _build(NSEQ, DEPTH, dbg, stop, final=True):
    nc = bass.Bass("TRN2", target_bir_lowering=False)

    def dram(name, shape, dtype, kind):
        return nc.dram_tensor(name, shape, dtype, kind=kind).ap()

    x_d = dram("x", [NSEQ, SL, DM], F32, "ExternalInput")
    pos_d = dram("positions", [NSEQ, SL], I32, "ExternalInput")
    gmix_d = dram("g_mix", [DEPTH, DM], F32, "ExternalInput")
    win_d = dram("w_in", [DEPTH, DM, NCOL], F32, "ExternalInput")
    bf_d = dram("b_forget", [DEPTH, 8], F32, "ExternalInput")
    wbr_d = dram("w_branch", [DEPTH, 3, 512, DM], F32, "ExternalInput")
    wout_d = dram("w_out", [DEPTH, DM, DM], F32, "ExternalInput")
    gmlp_d = dram("g_mlp", [DEPTH, DM], F32, "ExternalInput")
    wup_d = dram("w_up", [DEPTH, DM, 4 * DM], F32, "ExternalInput")
    wdn_d = dram("w_down", [DEPTH, 4 * DM, DM], F32, "ExternalInput")
    gfin_d = dram("g_final", [DM], F32, "ExternalInput")
    out_d = dram("out", [NSEQ, SL, DM], F32, "ExternalOutput")
    winb = dram("winb", [2, DM, NCOL], BF16, "Internal")
    wbrb = dram("wbrb", [2, 3, 512, DM], BF16, "Internal")
    woutb = dram("woutb", [2, DM, DM], BF16, "Internal")
    wupb = dram("wupb", [2, DM, 4 * DM], BF16, "Internal")
    wdnb = dram("wdnb", [2, 4 * DM, DM], BF16, "Internal")
    xs_d = dram("xs", [NSEQ, KC, 128, SL], F32, "Internal")
    aug_d = dram("augd", [3, 8, SL], BF16, "Internal")
    if dbg:
        dbg_h = dram("dbg_h", [128, KC, SL], F32, "ExternalOutput")
        dbg_br = dram("dbg_br", [3, 128, 4, SL], F32, "ExternalOutput")
        dbg_x = dram("dbg_x", [KC, 128, SL], F32, "ExternalOutput")

    top = ExitStack()
    S = Sch(nc)
    uid = [0]

    cur = [16384 + 256]
    peak = [0]
    LIMIT = 229376

    def sb(st, shape, dtype, name=None):
        uid[0] += 1
        nm = "%s_%d" % (name or 't', uid[0])
        isz = 2 if dtype == BF16 else 4
        nb = int(np.prod(shape[1:])) * isz
        nb = (nb + 63) // 64 * 64
        off = cur[0]
        cur[0] += nb
        peak[0] = max(peak[0], cur[0])
        assert cur[0] <= LIMIT, ("SBUF overflow", nm, cur[0])
        st.callback(lambda off=off: cur.__setitem__(0, off))
        return Tl(nc.alloc_sbuf_tensor_at(nm, shape, dtype, offset=off), nm)

    def bl(ts):
        return [t.b if isinstance(t, Tl) else t for t in ts]

    def mm(out, lhsT, rhs, start, stop, r, w):
        S.op('pe', lambda e: e.matmul(out, lhsT=lhsT, rhs=rhs, start=start, stop=stop), bl(r), bl(w))

    def tr(out, in_, ident, r, w):
        S.op('pe', lambda e: e.transpose(out=out, in_=in_, identity=ident), bl(r), bl(w))

    def act(out, in_, func, r, w, bias=None, scale=None):
        kw = {}
        if bias is not None:
            kw['bias'] = bias
        if scale is not None:
            kw['scale'] = scale
        S.op('act', lambda e: e.activation(out=out, in_=in_, func=func, **kw), bl(r), bl(w))

    def tt(eng, out, in0, in1, op, r, w):
        S.op(eng, lambda e: e.tensor_tensor(out=out, in0=in0, in1=in1, op=op), bl(r), bl(w))

    def ts(eng, out, in0, s1, s2, op0, op1, r, w, accum=None):
        kw = {}
        if op1 is not None:
            kw['op1'] = op1
        if accum is not None:
            kw['accum_out'] = accum
        S.op(eng, lambda e: e.tensor_scalar(out=out, in0=in0, scalar1=s1, scalar2=s2, op0=op0, **kw), bl(r), bl(w))

    def stt(eng, out, in0, scalar, in1, op0, op1, r, w):
        S.op(eng, lambda e: e.scalar_tensor_tensor(out=out, in0=in0, scalar=scalar, in1=in1, op0=op0, op1=op1), bl(r), bl(w))

    def cpy(eng, out, in_, r, w):
        if eng == 'act':
            S.op('act', lambda e: e.activation(out=out, in_=in_, func=AF.Copy), bl(r), bl(w))
        else:
            S.op(eng, lambda e: e.tensor_copy(out=out, in_=in_), bl(r), bl(w))

    def mset(eng, ap, val, w):
        S.op(eng, lambda e: e.memset(ap, val), [], bl(w))

    def asel(out, in_, pattern, cmp, fill, base, cm, r, w):
        S.op('pool', lambda e: e.affine_select(out=out, in_=in_, pattern=pattern, compare_op=cmp, fill=fill,
                                               base=base, channel_multiplier=cm), bl(r), bl(w))

    def dma(out, in_, r, w, q='sync'):
        S.dma(lambda e: e.dma_start(out=out, in_=in_), bl(r), bl(w), q=q)

    PS = [Tl(top.enter_context(nc.psum_tensor("ps%d" % i, [128, 512], F32)), "ps%d" % i) for i in range(8)]

    def psb(i):
        return PS[i].t[:].bitcast(BF16)

    identF = sb(top, [128, 128], F32, "identF")
    identB = sb(top, [128, 128], BF16, "identB")
    onesF = sb(top, [128, 128], F32, "onesF")
    onesB = sb(top, [128, 128], BF16, "onesB")
    triU = sb(top, [128, 128], BF16, "triU")
    opad = [sb(top, [128, 128], BF16, "opad%d" % i) for i in range(2)]
    ones3 = sb(top, [3, 128], BF16, "ones3")
    gst = sb(top, [40, 128], F32, "gst")
    gT = sb(top, [128, 40], F32, "gT")
    invf = sb(top, [128, 8], F32, "invf")
    bfneg = sb(top, [8, 2], F32, "bfneg")
    pow2 = sb(top, [128, NBIS + 1], F32, "pow2")
    tauc = sb(top, [128, 1], F32, "tauc")
    rden = sb(top, [128, 512], F32, "rden")
    BIG = 30000.0
    cmask = sb(top, [128, 128], F32, "cmask")
    mset('pool', cmask[:], 0.0, [cmask])
    asel(cmask[:], cmask[:], [[-1, 128]], ALU.is_ge, NEG, 0, 1, [cmask], [cmask])
    maskS = [sb(top, [128, 512], BF16, "maskS%d" % j) for j in range(4)]
    posA = [sb(top, [128, 512], BF16, "posA%d" % j) for j in range(4)]
    negB = [sb(top, [128, 512], BF16, "negB%d" % j) for j in range(4)]
    for j in range(4):
        mset('pool', maskS[j][:], 1.0, [maskS[j]])
        asel(maskS[j][:], maskS[j][:], [[1, 512]], ALU.is_gt, 0.0, -128 * j, -1, [maskS[j]], [maskS[j]])
        mset('pool', posA[j][:], 0.0, [posA[j]])
        asel(posA[j][:], posA[j][:], [[1, 512]], ALU.is_gt, BIG, -128 * j, -1, [posA[j]], [posA[j]])
        mset('pool', negB[j][:], 0.0, [negB[j]])
        asel(negB[j][:], negB[j][:], [[1, 512]], ALU.is_ge, -BIG, -128 * j, -1, [negB[j]], [negB[j]])

    for t_, dtv in ((identF, 0.0), (identB, 0.0)):
        mset('pool', t_[:], 0.0, [t_])
        asel(t_[:], t_[:], [[-1, 128]], ALU.not_equal, 1.0, 0, 1, [t_], [t_])
    mset('pool', onesF[:], 1.0, [onesF])
    mset('pool', onesB[:], 1.0, [onesB])
    mset('pool', ones3[:], 1.0, [ones3])
    mset('pool', triU[:], 1.0, [triU])
    asel(triU[:], triU[:], [[-1, 128]], ALU.is_gt, 0.0, 0, 1, [triU], [triU])
    for i in range(2):
        mset('pool', opad[i][:], 0.0, [opad[i]])
        mset('pool', opad[i][:, i * 64:(i + 1) * 64], 1.0, [opad[i]])
    invfreq = (np.float32(500000.0) ** (-(np.arange(0, 16, 2, dtype=np.float32)) / np.float32(16))).astype(np.float32)
    for j in range(8):
        mset('pool', invf[:, j:j + 1], float(invfreq[j]), [invf])
    for k in range(NBIS + 1):
        mset('pool', pow2[:, k:k + 1], float(2.0 ** -(k + 1)), [pow2])
    mset('pool', tauc[:], -1.0e29, [tauc])
    for r_, src in ((0, gmix_d[0]), (1, gmix_d[DEPTH - 1]), (2, gmlp_d[0]), (3, gmlp_d[DEPTH - 1]), (4, gfin_d)):
        dma(gst[r_ * 8:(r_ + 1) * 8, :], src.rearrange("(c p) -> c p", p=128), [], [gst])
    tr(PS[0][:, 0:40], gst[0:40, :], identF[0:40, 0:40], [gst, identF], [PS[0]])
    cpy('dve', gT[:], PS[0][:, 0:40], [PS[0]], [gT])
    for l in range(DEPTH):
        dma(bfneg[:, l:l + 1], bf_d[l].rearrange("(h o) -> h o", o=1), [], [bfneg])
    ts('dve', bfneg[:, 0:DEPTH], bfneg[:, 0:DEPTH], -1.0, None, ALU.mult, None, [bfneg], [bfneg])

    wbuf = {}

    def cast2d(dst, src, rows, key):
        bs = []
        for r0 in range(0, rows, 128):
            b = Buf('%s_%d' % (str(key), r0))
            bs.append(b)
            dma(dst[r0:r0 + 128, :], src[r0:r0 + 128, :], [], [b], q='pool')
        wbuf[key] = bs

    def cast_layer(l):
        cast2d(winb[l], win_d[l], DM, ('in', l))
        cast2d(wbrb[l].rearrange("b k f -> (b k) f"), wbr_d[l].rearrange("b k f -> (b k) f"), 1536, ('br', l))
        cast2d(woutb[l], wout_d[l], DM, ('out', l))
        cast2d(wupb[l], wup_d[l], DM, ('up', l))
        cast2d(wdnb[l], wdn_d[l], 4 * DM, ('dn', l))

    cast_layer(0)
    cast_done = [1]

    def wload(tile, dst_ap, key, src2d, c0, c1):
        dma(dst_ap, src2d[:, c0:c1].rearrange("(kc p) f -> p kc f", p=128), wbuf[key], [tile])

    def norm_block(st_tmp, xblk, gidx, outs, out_bufs, ps_i, tmp):
        sq, rs, rs2 = tmp
        act(sq[:], xblk[:], AF.Square, [xblk], [sq])
        for c in range(KC):
            mm(PS[ps_i][:], onesF[:], sq[:, c, :], c == 0, c == KC - 1, [onesF, sq], [PS[ps_i]])
        ts('dve', rs[:], PS[ps_i][:], 1.0 / DM, 1e-6, ALU.mult, ALU.add, [PS[ps_i]], [rs])
        act(rs2[:], rs[:], AF.Sqrt, [rs], [rs2])
        S.op('dve', lambda e: e.reciprocal(out=rs[:], in_=rs2[:]), bl([rs2]), bl([rs]))
        for c in range(KC):
            stt('dve', outs(c), xblk[:, c, :], gT[:, gidx * 8 + c:gidx * 8 + c + 1], rs[:], ALU.mult, ALU.mult,
                [xblk, gT, rs], out_bufs)

    def run_chains(chains):
        chains = list(chains)
        while chains:
            nxt = []
            for c in chains:
                try:
                    next(c)
                    nxt.append(c)
                except StopIteration:
                    pass
            chains = nxt

    evac_rr = [0]

    def evac(out, in_, r, w):
        evac_rr[0] += 1
        if evac_rr[0] % 2:
            cpy('act', out, in_, r, w)
        else:
            cpy('dve', out, in_, r, w)

    slc = [0]

    def chk(k):
        if stop <= k + 10 * slc[0]:
            raise StopBuild()

    def body():
      for s in range(NSEQ):
        seqst = ExitStack()
        cosT = sb(seqst, [128, NT, 8], F32, "cosT")
        sinT = sb(seqst, [128, NT, 8], F32, "sinT")
        with ExitStack() as st:
            xtm = [sb(st, [128, DM], F32, "xtm") for _ in range(2)]
            xblk = [sb(st, [128, KC, 512], F32, "xblk") for _ in range(2)]
            xsb = Buf('xs')
            for tb in range(NB):
                xb_ = xblk[tb % 2]
                for t4 in range(4):
                    tt_ = tb * 4 + t4
                    xt = xtm[tt_ % 2]
                    dma(xt[:], x_d[s][tt_ * 128:(tt_ + 1) * 128, :], [], [xt])
                    for half in range(2):
                        pb = PS[half]
                        for c4 in range(4):
                            c = half * 4 + c4
                            tr(pb[:, c4 * 128:(c4 + 1) * 128], xt[:, c * 128:(c + 1) * 128], identF[:],
                               [xt, identF], [pb])
                        cpy('dve', xb_[:, half * 4:(half + 1) * 4, t4 * 128:(t4 + 1) * 128],
                            pb[:].rearrange("p (c t) -> p c t", t=128), [pb], [xb_])
                dma(xs_d[s][:, :, tb * 512:(tb + 1) * 512].rearrange("c p t -> p c t"), xb_[:], [xb_], [xsb])
            posi = sb(st, [16, 128], I32, "posi")
            posf = sb(st, [16, 128], F32, "posf")
            posT = sb(st, [128, 16], F32, "posT")
            ang = sb(st, [128, NT, 8], F32, "ang")
            kf = sb(st, [128, NT, 8], F32, "kf")
            ki_ = sb(st, [128, NT, 8], I32, "ki")
            dma(posi[:], pos_d[s].rearrange("(t p) -> t p", p=128), [], [posi])
            cpy('dve', posf[:], posi[:], [posi], [posf])
            tr(PS[2][:, 0:16], posf[0:16, :], identF[0:16, 0:16], [posf, identF], [PS[2]])
            cpy('dve', posT[:], PS[2][:, 0:16], [PS[2]], [posT])
            for tab, shift in ((sinT, 0.0), (cosT, PI / 2)):
                tt('dve', ang[:], posT[:].unsqueeze(2).to_broadcast([128, NT, 8]),
                   invf[:].unsqueeze(1).to_broadcast([128, NT, 8]), ALU.mult, [posT, invf], [ang])
                if shift:
                    ts('dve', ang[:], ang[:], shift, None, ALU.add, None, [ang], [ang])
                ts('dve', kf[:], ang[:], 1.0 / TWO_PI, None, ALU.mult, None, [ang], [kf])
                cpy('dve', ki_[:], kf[:], [kf], [ki_])
                cpy('dve', kf[:], ki_[:], [ki_], [kf])
                stt('dve', ang[:], kf[:], -TWO_PI, ang[:], ALU.mult, ALU.add, [kf, ang], [ang])
                ts('dve', kf[:], ang[:], PI, TWO_PI, ALU.is_gt, ALU.mult, [ang], [kf])
                tt('dve', ang[:], ang[:], kf[:], ALU.subtract, [ang, kf], [ang])
                ts('dve', kf[:], ang[:], -PI, TWO_PI, ALU.is_lt, ALU.mult, [ang], [kf])
                tt('dve', ang[:], ang[:], kf[:], ALU.add, [ang, kf], [ang])
                act(tab[:], ang[:], AF.Sin, [ang], [tab])
        S.fence()
        chk(1)

        for l in range(DEPTH):
            slc[0] = s * DEPTH + l
            last_layer = (l == DEPTH - 1)
            lay = ExitStack()
            hT = sb(lay, [128, KC, SL], BF16, "hT")
            with ExitStack() as st:
                xblk = [sb(st, [128, KC, 512], F32, "xblk") for _ in range(2)]
                sq = sb(st, [128, KC, 512], F32, "sq")
                rs = sb(st, [128, 512], F32, "rs")
                rs2 = sb(st, [128, 512], F32, "rs2")
                for tb in range(NB):
                    xb_ = xblk[tb % 2]
                    dma(xb_[:], xs_d[s][:, :, tb * 512:(tb + 1) * 512].rearrange("c p t -> p c t"), [xsb], [xb_])
                    norm_block(st, xb_, l, lambda c, tb=tb: hT[:, c, tb * 512:(tb + 1) * 512], [hT], 0, (sq, rs, rs2))
            S.fence()
            if dbg and s == 0 and l == 0:
                dma(dbg_h, hT[:], [hT], [], q='pool')
            chk(2)

            inner = ExitStack()
            brT = [sb(inner, [128, 4, SL], BF16, "brT%d" % b) for b in range(3)]

            def proj_fm(wt, col0, dst_fn, dst, pbanks):
                for tb in range(NB):
                    pb = PS[pbanks[tb % 2]]
                    for kc in range(KC):
                        mm(pb[:], wt[:, kc, col0:col0 + 128], hT[:, kc, tb * 512:(tb + 1) * 512], kc == 0, kc == KC - 1,
                           [wt, hT], [pb])
                    evac(dst_fn(tb), pb[:], [pb], [dst])
                    yield

            def proj_vpad(wt, col0, vp, pbanks):
                for g4 in range(4):
                    pb = PS[pbanks[g4 % 2]]
                    for t4 in range(4):
                        tt_ = g4 * 4 + t4
                        for kc in range(KC):
                            mm(pb[:, t4 * 128:(t4 + 1) * 128], hT[:, kc, tt_ * 128:(tt_ + 1) * 128],
                               wt[:, kc, col0:col0 + 128], kc == 0, kc == KC - 1, [wt, hT], [pb])
                    pv = pb[:].rearrange("p (t f) -> p t f", f=128)
                    for hl in range(2):
                        cpy('dve', vp[hl][:, g4 * 4:(g4 + 1) * 4, hl * 64:(hl + 1) * 64], pv[:, :, hl * 64:(hl + 1) * 64],
                            [pb], [vp[hl]])
                    yield

            def proj_hp(wq, wk, wv, hp, qT, kT, vp, extra=None):
                yield from proj_fm(wq, hp * 128, lambda tb: qT[:, tb * 512:(tb + 1) * 512], qT, (0, 1))
                yield from proj_fm(wk, hp * 128, lambda tb: kT[:, tb * 512:(tb + 1) * 512], kT, (0, 1))
                yield from proj_vpad(wv, hp * 128, vp, (0, 1))
                if extra is not None:
                    extra()

            with ExitStack() as st:
                wq = sb(st, [128, KC, 512], BF16, "wq"); wk = sb(st, [128, KC, 512], BF16, "wk")
                wv = sb(st, [128, KC, 512], BF16, "wv")
                for wt, c0 in ((wq, QA), (wk, KA), (wv, VA)):
                    wload(wt, wt[:], ('in', l), winb[l], c0, c0 + 512)
                qTp = [sb(st, [128, SL], BF16, "qTp") for _ in range(2)]
                kTp = [sb(st, [128, SL], BF16, "kTp") for _ in range(2)]
                vpd = [[sb(st, [128, NT, 128], BF16, "vpd") for _ in range(2)] for _ in range(2)]
                for a in vpd:
                    for v_ in a:
                        mset('pool', v_[:], 0.0, [v_])
                W = []
                for ch in range(2):
                    W.append(dict(e=sb(st, [128, 512], F32, "e"), sp=sb(st, [128, 512], F32, "sp"),
                                  nl=sb(st, [128, 512], BF16, "nl"), R=sb(st, [128, 512], F32, "R"),
                                  Rb=sb(st, [128, 512], BF16, "Rb"), tsum=sb(st, [128, 512], F32, "tsum"),
                                  wT=sb(st, [128, 512], BF16, "wT"), z=PS[2 + ch], lat=PS[4 + ch]))

                def chainA(hl, qT, kT, vp, Q, acc, w_):
                    base = hl * 64
                    nk = 4 * Q + 4
                    for idx, kt in enumerate(range(nk - 1, -1, -1)):
                        diag = kt >= 4 * Q
                        j = kt - 4 * Q
                        mm(w_['z'][:], kT[base:base + 64, kt * 128:(kt + 1) * 128],
                           qT[base:base + 64, Q * 512:(Q + 1) * 512], True, True, [kT, qT], [w_['z']])
                        yield
                        act(w_['e'][:], w_['z'][:], AF.Exp, [w_['z']], [w_['e']], scale=-SCALE)
                        yield
                        act(w_['sp'][:], w_['e'][:], AF.Ln, [w_['e']], [w_['sp']], bias=1.0)
                        yield
                        stt('dve', w_['nl'][:], w_['z'][:], SCALE, w_['sp'][:], ALU.mult, ALU.add,
                            [w_['z'], w_['sp']], [w_['nl']])
                        yield
                        if diag:
                            tt('pool', w_['nl'][:], w_['nl'][:], maskS[j][:], ALU.mult, [w_['nl'], maskS[j]], [w_['nl']])
                            yield
                        last_lat = (idx == 0) and not diag
                        mm(w_['lat'][:], triU[:], w_['nl'][:], True, (idx == 0 and not diag), [triU, w_['nl']], [w_['lat']])
                        if idx > 0:
                            mm(w_['lat'][:], onesB[:], w_['Rb'][:], False, not diag, [onesB, w_['Rb']], [w_['lat']])
                        if diag:
                            mm(w_['lat'][:], identB[:], posA[j][:], False, True, [identB, posA[j]], [w_['lat']])
                        yield
                        tt('dve', w_['tsum'][:], w_['lat'][:], w_['sp'][:], ALU.add, [w_['lat'], w_['sp']], [w_['tsum']])
                        yield
                        act(w_['wT'][:], w_['tsum'][:], AF.Exp, [w_['tsum']], [w_['wT']], scale=-1.0)
                        yield
                        mm(acc[:], vp[hl][:, kt, :], w_['wT'][:], hl == 0 and idx == 0, hl == 1 and idx == nk - 1,
                           [vp[hl], w_['wT']], [acc])
                        yield
                        if kt > 0:
                            if idx == 0:
                                cpy('pool', w_['R'][:], w_['nl'][:], [w_['nl']], [w_['R']])
                            else:
                                tt('pool', w_['R'][:], w_['R'][:], w_['nl'][:], ALU.add, [w_['R'], w_['nl']], [w_['R']])
                            yield
                            cpy('pool', w_['Rb'][:], w_['R'][:], [w_['R']], [w_['Rb']])
                            yield

                run_chains([proj_hp(wq, wk, wv, 0, qTp[0], kTp[0], vpd[0])])
                for hp in range(4):
                    i_ = hp % 2
                    for Q in range(NB):
                        acc = PS[6 + (Q % 2)]
                        chains = [chainA(hl, qTp[i_], kTp[i_], vpd[i_], Q, acc, W[hl]) for hl in range(2)]
                        if Q == NB - 1 and hp + 1 < 4:
                            n_ = (hp + 1) % 2
                            chains.append(proj_hp(wq, wk, wv, hp + 1, qTp[n_], kTp[n_], vpd[n_]))
                        run_chains(chains)
                        evac(brT[0][:, hp, Q * 512:(Q + 1) * 512], acc[:], [acc], [brT[0]])
            S.fence()
            if dbg and s == 0 and l == 0:
                dma(dbg_br[0], brT[0][:], [brT[0]], [], q='pool')
            chk(3)

            with ExitStack() as st:
                wq = sb(st, [128, KC, 512], BF16, "wq"); wk = sb(st, [128, KC, 512], BF16, "wk")
                wv = sb(st, [128, KC, 512], BF16, "wv")
                wfl = sb(st, [128, KC, 8], BF16, "wfl")
                for wt, c0 in ((wq, QB), (wk, KB), (wv, VB)):
                    wload(wt, wt[:], ('in', l), winb[l], c0, c0 + 512)
                wload(wfl, wfl[:], ('in', l), winb[l], FLC, FLC + 8)
                qTp = [sb(st, [128, SL], BF16, "qTp") for _ in range(2)]
                kTp = [sb(st, [128, SL], BF16, "kTp") for _ in range(2)]
                vpd = [[sb(st, [128, NT, 128], BF16, "vpd") for _ in range(2)] for _ in range(2)]
                for a in vpd:
                    for v_ in a:
                        mset('pool', v_[:], 0.0, [v_])
                augp = [sb(st, [3, 2, SL], BF16, "augp") for _ in range(2)]
                cnegT = sb(st, [128, NT, 8], F32, "cnegT")
                augb = Buf('augd')
                with ExitStack() as st2:
                    lfe = sb(st2, [8, SL], F32, "lfe")
                    cneg = sb(st2, [8, SL], F32, "cneg"); v8 = sb(st2, [8, SL], F32, "v8")
                    part = sb(st2, [8, SL], BF16, "part")
                    for tb in range(NB):
                        pb = PS[tb % 2]
                        for kc in range(KC):
                            mm(pb[0:8, :], wfl[:, kc, :], hT[:, kc, tb * 512:(tb + 1) * 512], kc == 0, kc == KC - 1,
                               [wfl, hT], [pb])
                        act(lfe[:, tb * 512:(tb + 1) * 512], pb[0:8, :], AF.Exp, [pb, bfneg], [lfe],
                            bias=bfneg[:, l:l + 1], scale=-1.0)
                    act(lfe[:], lfe[:], AF.Ln, [lfe], [lfe], bias=1.0)
                    S.op('dve', lambda e: e.tensor_tensor_scan(out=cneg[:], data0=lfe[:], data1=lfe[:], initial=0.0,
                                                               op0=ALU.add, op1=ALU.bypass), bl([lfe]), bl([cneg]))
                    ts('dve', v8[:], cneg[:], -8.0, None, ALU.mult, None, [cneg], [v8])
                    for pi_ in range(3):
                        cpy('dve', part[:], v8[:], [v8], [part])
                        if pi_ < 2:
                            tt('dve', v8[:], v8[:], part[:], ALU.subtract, [v8, part], [v8])
                        dma(aug_d[pi_], part[:], [part], [augb])
                    for tt_ in range(NT):
                        tr(PS[2][:, tt_ * 8:(tt_ + 1) * 8], cneg[0:8, tt_ * 128:(tt_ + 1) * 128], identF[0:8, 0:8],
                           [cneg, identF], [PS[2]])
                    cpy('dve', cnegT[:], PS[2][:, 0:128].rearrange("p (t h) -> p t h", h=8), [PS[2]], [cnegT])
                S.fence()
                W = []
                for ch in range(4):
                    W.append(dict(pT=sb(st, [128, 512], BF16, "pT"), z=PS[(2, 3, 0, 1)[ch]]))

                def chainB(hl, h, qT, kT, vp, aq, Q, num, den, w_):
                    base = hl * 64
                    nk = 4 * Q + 4
                    for idx, kt in enumerate(range(nk)):
                        diag = kt >= 4 * Q
                        j = kt - 4 * Q
                        mm(w_['z'][:], kT[base:base + 64, kt * 128:(kt + 1) * 128],
                           qT[base:base + 64, Q * 512:(Q + 1) * 512], True, False, [kT, qT], [w_['z']])
                        mm(w_['z'][:], ones3[0:3, :], aq[0:3, hl, Q * 512:(Q + 1) * 512], False, not diag,
                           [ones3, aq], [w_['z']])
                        if diag:
                            mm(w_['z'][:], identB[:], negB[j][:], False, True, [identB, negB[j]], [w_['z']])
                        yield
                        act(w_['pT'][:], w_['z'][:], AF.Exp, [w_['z'], cnegT], [w_['pT']],
                            bias=cnegT[:, kt, h:h + 1], scale=SCALE)
                        yield
                        first = (hl == 0 and idx == 0)
                        lastf = (hl == 1 and idx == nk - 1)
                        mm(num[:], vp[hl][:, kt, :], w_['pT'][:], first, lastf, [vp[hl], w_['pT']], [num])
                        mm(den[:], opad[hl][:], w_['pT'][:], first, lastf, [opad[hl], w_['pT']], [den])
                        yield

                def aug_load(hp_, buf_):
                    return lambda: dma(buf_[:], aug_d[:, 2 * hp_:2 * hp_ + 2, :], [augb], [buf_])

                for hpp in range(2):
                    for k in range(2):
                        hp = 2 * hpp + k
                        run_chains([proj_hp(wq, wk, wv, hp, qTp[k], kTp[k], vpd[k], aug_load(hp, augp[k]))])
                    for Q in range(NB):
                        chains = []
                        for k in range(2):
                            hp = 2 * hpp + k
                            for hl in range(2):
                                chains.append(chainB(hl, 2 * hp + hl, qTp[k], kTp[k], vpd[k], augp[k], Q,
                                                     PS[4 + 2 * k], PS[5 + 2 * k], W[2 * k + hl]))
                        run_chains(chains)
                        for k in range(2):
                            hp = 2 * hpp + k
                            num, den = PS[4 + 2 * k], PS[5 + 2 * k]
                            S.op('dve', lambda e, den=den: e.reciprocal(out=rden[:], in_=den[:]), bl([den]), bl([rden]))
                            tt('dve', brT[1][:, hp, Q * 512:(Q + 1) * 512], num[:], rden[:], ALU.mult, [num, rden], [brT[1]])
            S.fence()
            if dbg and s == 0 and l == 0:
                dma(dbg_br[1], brT[1][:], [brT[1]], [], q='pool')
            chk(4)

            with ExitStack() as st:
                qcT = sb(st, [128, 4, SL], BF16, "qcT")
                kcTd = sb(st, [128, 2, SL], BF16, "kcTd")
                qiT = sb(st, [128, 2, SL], BF16, "qiT")
                kiT2 = sb(st, [128, SL], BF16, "kiT2")
                vpc = [[sb(st, [128, NT, 128], BF16, "vpc") for _ in range(2)] for _ in range(2)]
                wiT = sb(st, [128, NT, 4], F32, "wiT")
                for a in vpc:
                    for v_ in a:
                        mset('pool', v_[:], 0.0, [v_])
                with ExitStack() as st2:
                    wc = sb(st2, [128, KC, CW], BF16, "wc")
                    wload(wc, wc[:], ('in', l), winb[l], CC, CC + CW)
                    cq = sb(st2, [128, 512], F32, "cq")
                    c1 = sb(st2, [128, 512], F32, "c1")
                    c2 = sb(st2, [128, 128], F32, "c2")
                    kd = sb(st2, [128, 128], F32, "kd")
                    kcd = sb(st2, [128, 2, 128], F32, "kcd")
                    rt = [sb(st2, [128, 8, 8], F32, "rt") for _ in range(4)]

                    def rope(t_, ncol, nh, tt_):
                        dv = t_[:, 0:ncol].rearrange("p (h d) -> p h d", d=64)
                        cs = cosT[:, tt_, :].unsqueeze(1).to_broadcast([128, nh, 8])
                        sn = sinT[:, tt_, :].unsqueeze(1).to_broadcast([128, nh, 8])
                        x1 = dv[:, :, 0:8]
                        x2 = dv[:, :, 8:16]
                        r0, r1, r2, r3 = [r_[:, 0:nh, :] for r_ in rt]
                        tt('dve', r0, x1, cs, ALU.mult, [t_, cosT], [rt[0]])
                        tt('dve', r1, x2, sn, ALU.mult, [t_, sinT], [rt[1]])
                        tt('dve', r2, x2, cs, ALU.mult, [t_, cosT], [rt[2]])
                        tt('dve', r3, x1, sn, ALU.mult, [t_, sinT], [rt[3]])
                        tt('dve', x1, r0, r1, ALU.subtract, [rt[0], rt[1], t_], [t_])
                        tt('dve', x2, r2, r3, ALU.add, [rt[2], rt[3], t_], [t_])

                    for tt_ in range(NT):
                        tsl = slice(tt_ * 128, (tt_ + 1) * 128)
                        for bi, (c0, cw_) in enumerate(((0, 512), (512, 512), (1024, 68))):
                            pb = PS[bi]
                            for kc in range(KC):
                                mm(pb[:, 0:cw_], hT[:, kc, tsl], wc[:, kc, c0:c0 + cw_], kc == 0, kc == KC - 1,
                                   [wc, hT], [pb])
                        cpy('act', cq[:], PS[0][:], [PS[0]], [cq])
                        cpy('act', c1[:], PS[1][:], [PS[1]], [c1])
                        cpy('dve', c2[:, 0:68], PS[2][:, 0:68], [PS[2]], [c2])
                        rope(cq, 512, 8, tt_)
                        rope(c1, 512, 8, tt_)
                        rope(c2, 64, 1, tt_)
                        cpy('pool', kd[:, 0:64], c2[:, 0:64], [c2], [kd])
                        cpy('pool', kd[:, 64:128], c2[:, 0:64], [c2], [kd])
                        ts('dve', wiT[:, tt_, :], c2[:, 64:68], 0.5, None, ALU.mult, None, [c2], [wiT])
                        for g in range(2):
                            for half in range(2):
                                cpy('dve', vpc[g][half][:, tt_, half * 64:(half + 1) * 64],
                                    PS[1][:, 128 + g * 64:128 + (g + 1) * 64], [PS[1]], [vpc[g][half]])
                                cpy('pool', kcd[:, g, half * 64:(half + 1) * 64], c1[:, g * 64:(g + 1) * 64],
                                    [c1], [kcd])
                        for c in range(4):
                            tr(PS[3][:, c * 128:(c + 1) * 128], cq[:, c * 128:(c + 1) * 128], identF[:],
                               [cq, identF], [PS[3]])
                        cpy('dve', qcT[:, :, tsl], PS[3][:].rearrange("p (c t) -> p c t", t=128), [PS[3]], [qcT])
                        for g in range(2):
                            tr(PS[4][:, g * 128:(g + 1) * 128], kcd[:, g, :], identF[:], [kcd, identF], [PS[4]])
                        for c in range(2):
                            tr(PS[4][:, (2 + c) * 128:(3 + c) * 128], c1[:, 256 + c * 128:256 + (c + 1) * 128], identF[:],
                               [c1, identF], [PS[4]])
                        tr(PS[5][:, 0:128], kd[:], identF[:], [kd, identF], [PS[5]])
                        cpy('dve', kcTd[:, :, tsl], PS[4][:, 0:256].rearrange("p (c t) -> p c t", t=128), [PS[4]], [kcTd])
                        cpy('dve', qiT[:, :, tsl], PS[4][:, 256:512].rearrange("p (c t) -> p c t", t=128), [PS[4]], [qiT])
                        cpy('dve', kiT2[:, tsl], PS[5][:, 0:128], [PS[5]], [kiT2])
                S.fence()
                chk(4.3)
                with ExitStack() as st3:
                    sc = [sb(st3, [128, SL], F32, "sc") for _ in range(2)]
                    junk2 = [sb(st3, [128, SL], BF16, "junk") for _ in range(2)]
                    mk = [sb(st3, [128, SL], F32, "mk") for _ in range(1)]
                    mkT = [sb(st3, [128, NT, 512], BF16, "mkT") for _ in range(1)]
                    rl = [sb(st3, [128, 512], F32, "rl") for _ in range(1)]
                    BST = [dict(mx=sb(st3, [128, 1], F32, "mx"), mn=sb(st3, [128, 1], F32, "mn"),
                                wd=sb(st3, [128, 1], F32, "wd"), hs=sb(st3, [128, NBIS + 1], F32, "hs"),
                                mids=sb(st3, [128, NBIS + 1], F32, "mids"), cnts=sb(st3, [128, NBIS], F32, "cnts"),
                                gg=sb(st3, [128, 1], F32, "gg")) for _ in range(2)]
                    W = []
                    for ch in range(4):
                        W.append(dict(pT=sb(st3, [128, 512], BF16, "pT"), z=PS[(4, 5, 0, 1)[ch]]))
                    rlc = [0]

                    def indexer(i):
                        Q = i // 4
                        j = i % 4
                        n = (i + 1) * 128
                        nfull = (4 * Q + 4) * 128
                        sc_ = sc[i % 2]
                        b_ = BST[i % 2]
                        mx, mn, wd, hs, mids, cnts, gg = (b_['mx'], b_['mn'], b_['wd'], b_['hs'], b_['mids'],
                                                          b_['cnts'], b_['gg'])
                        junk = junk2[i % 2]
                        mk_ = mk[0]
                        qsl = slice(i * 128, (i + 1) * 128)
                        for c0 in range(0, n, 512):
                            cw_ = min(512, n - c0)
                            for h in range(4):
                                pb = PS[rlc[0] % 2]
                                r_ = rl[0]
                                rlc[0] += 1
                                hb = (h % 2) * 64
                                mm(pb[:, 0:cw_], qiT[hb:hb + 64, h // 2, qsl], kiT2[hb:hb + 64, c0:c0 + cw_], True, True,
                                   [qiT, kiT2], [pb])
                                act(r_[:, 0:cw_], pb[:, 0:cw_], AF.Relu, [pb], [r_], scale=0.125)
                                if h == 0:
                                    ts('dve', sc_[:, c0:c0 + cw_], r_[:, 0:cw_], wiT[:, i, 0:1], None, ALU.mult, None,
                                       [r_, wiT], [sc_])
                                else:
                                    stt('dve', sc_[:, c0:c0 + cw_], r_[:, 0:cw_], wiT[:, i, h:h + 1], sc_[:, c0:c0 + cw_],
                                        ALU.mult, ALU.add, [r_, wiT, sc_], [sc_])
                                yield
                        if i >= 2:
                            S.op('dve', lambda e: e.tensor_reduce(out=mx[:], in_=sc_[:, 0:n], axis=AX.X, op=ALU.max),
                                 bl([sc_]), bl([mx]))
                            S.op('dve', lambda e: e.tensor_reduce(out=mn[:], in_=sc_[:, 0:n], axis=AX.X, op=ALU.min),
                                 bl([sc_]), bl([mn]))
                        tt('pool', sc_[:, qsl], sc_[:, qsl], cmask[:], ALU.add, [sc_, cmask], [sc_])
                        if n < nfull:
                            mset('pool', sc_[:, n:nfull], NEG, [sc_])
                        if i >= 2:
                            stt('dve', wd[:], mx[:], 1.0, mn[:], ALU.add, ALU.subtract, [mx, mn], [wd])
                            ts('dve', hs[:], pow2[:], wd[:, 0:1], None, ALU.mult, None, [pow2, wd], [hs])
                            tt('dve', mids[:, 0:1], mn[:], hs[:, 0:1], ALU.add, [mn, hs], [mids])
                            mset('dve', cnts[:], 0.0, [cnts])
                            yield
                            for k in range(NBIS):
                                ts('dve', junk[:, 0:n], sc_[:, 0:n], mids[:, k:k + 1], 0.0, ALU.is_ge, ALU.add,
                                   [sc_, mids], [junk, cnts], accum=cnts[:, k:k + 1])
                                yield
                                si = k + 1 if k < NBIS - 1 else k
                                ts('dve', gg[:], cnts[:, k:k + 1], 255.5, hs[:, k:k + 1], ALU.is_gt, ALU.mult,
                                   [cnts, hs], [gg])
                                yield
                                stt('dve', mids[:, k + 1:k + 2], gg[:], hs[:, si:si + 1], mids[:, k:k + 1],
                                    ALU.subtract, ALU.add, [gg, hs, mids], [mids])
                                yield
                            tau = mids[:, NBIS:NBIS + 1]
                            taub = mids
                        else:
                            tau = tauc[:, 0:1]
                            taub = tauc
                        ts('dve', mk_[:, 0:nfull], sc_[:, 0:nfull], tau, None, ALU.is_ge, None, [sc_, taub], [mk_])
                        mt = mkT[0]
                        nkt = 4 * Q + 4
                        for k0 in range(0, nkt, 4):
                            kn = min(4, nkt - k0)
                            pbi = 2 + ((k0 // 4) % 2)
                            for kk in range(kn):
                                kt = k0 + kk
                                tr(PS[pbi][:, kk * 128:(kk + 1) * 128], mk_[:, kt * 128:(kt + 1) * 128], identF[:],
                                   [mk_, identF], [PS[pbi]])
                            cpy('dve', mt[:, k0:k0 + kn, j * 128:(j + 1) * 128],
                                PS[pbi][:, 0:kn * 128].rearrange("p (k t) -> p k t", t=128), [PS[pbi]], [mt])

                    def chainC(head, Q, num, den, w_, first_chain, last_chain):
                        g = head // 4
                        half = head % 2
                        chunk = head // 2
                        base = half * 64
                        nk = 4 * Q + 4
                        mt = mkT[0]
                        for idx, kt in enumerate(range(nk)):
                            mm(w_['z'][:], kcTd[base:base + 64, g, kt * 128:(kt + 1) * 128],
                               qcT[base:base + 64, chunk, Q * 512:(Q + 1) * 512], True, True, [kcTd, qcT], [w_['z']])
                            yield
                            act(w_['pT'][:], w_['z'][:], AF.Exp, [w_['z']], [w_['pT']], scale=SCALE)
                            yield
                            tt('dve', w_['pT'][:], w_['pT'][:], mt[:, kt, :], ALU.mult, [w_['pT'], mt], [w_['pT']])
                            yield
                            first = first_chain and idx == 0
                            lastf = last_chain and idx == nk - 1
                            mm(num[:], vpc[g][half][:, kt, :], w_['pT'][:], first, lastf, [vpc[g][half], w_['pT']], [num])
                            mm(den[:], opad[half][:], w_['pT'][:], first, lastf, [opad[half], w_['pT']], [den])
                            yield

                    for Q in range(NB):
                        for j in range(0, 4, 2):
                            run_chains([indexer(4 * Q + j), indexer(4 * Q + j + 1)])
                            chk(4.4 if j < 2 else 4.5)
                        chk(4.6)
                        for hpp in range(2):
                            chains = []
                            for k in range(2):
                                hp = 2 * hpp + k
                                num, den = (PS[6], PS[7]) if k == 0 else (PS[2], PS[3])
                                for hl in range(2):
                                    chains.append(chainC(2 * hp + hl, Q, num, den, W[2 * k + hl], hl == 0, hl == 1))
                            run_chains(chains)
                            for k in range(2):
                                hp = 2 * hpp + k
                                num, den = (PS[6], PS[7]) if k == 0 else (PS[2], PS[3])
                                S.op('dve', lambda e, den=den: e.reciprocal(out=rden[:], in_=den[:]), bl([den]), bl([rden]))
                                tt('dve', brT[2][:, hp, Q * 512:(Q + 1) * 512], num[:], rden[:], ALU.mult, [num, rden], [brT[2]])
                                chk(4.7)
            S.fence()
            if dbg and s == 0 and l == 0:
                dma(dbg_br[2], brT[2][:], [brT[2]], [], q='pool')
            chk(5)

            with ExitStack() as st:
                yT = sb(st, [128, KC, SL], BF16, "yT")
                wb_ = [sb(st, [128, 4, 512], BF16, "wb") for _ in range(3)]
                wg_ = [sb(st, [128, KC, 512], BF16, "wg") for _ in range(3)]
                sg = [sb(st, [128, 512], F32, "sg") for _ in range(2)]
                accy = sb(st, [128, 512], F32, "accy")
                tmpy = sb(st, [128, 512], F32, "tmpy")
                pc_ = [0]
                for fg in range(2):
                    for b in range(3):
                        dma(wb_[b][:], wbrb[l][b][:, fg * 512:(fg + 1) * 512].rearrange("(kc p) f -> p kc f", p=128),
                            wbuf[('br', l)], [wb_[b]])
                        wload(wg_[b], wg_[b][:], ('in', l), winb[l], GATE + b * DM + fg * 512, GATE + b * DM + (fg + 1) * 512)
                    for tb in range(NB):
                        tsl = slice(tb * 512, (tb + 1) * 512)
                        for fc in range(4):
                            c = fg * 4 + fc
                            fsl = slice(fc * 128, (fc + 1) * 128)
                            for b in range(3):
                                pp = PS[(pc_[0] % 4) * 2]
                                pg = PS[(pc_[0] % 4) * 2 + 1]
                                sg_ = sg[pc_[0] % 2]
                                pc_[0] += 1
                                for kc in range(KC):
                                    mm(pg[:], wg_[b][:, kc, fsl], hT[:, kc, tsl], kc == 0, kc == KC - 1, [wg_[b], hT], [pg])
                                for k4 in range(4):
                                    mm(pp[:], wb_[b][:, k4, fsl], brT[b][:, k4, tsl], k4 == 0, k4 == 3, [wb_[b], brT[b]], [pp])
                                act(sg_[:], pg[:], AF.Sigmoid, [pg], [sg_])
                                if b == 0:
                                    tt('dve', accy[:], pp[:], sg_[:], ALU.mult, [pp, sg_], [accy])
                                elif b == 1:
                                    tt('dve', tmpy[:], pp[:], sg_[:], ALU.mult, [pp, sg_], [tmpy])
                                    tt('pool', accy[:], accy[:], tmpy[:], ALU.add, [accy, tmpy], [accy])
                                else:
                                    tt('dve', tmpy[:], pp[:], sg_[:], ALU.mult, [pp, sg_], [tmpy])
                                    tt('pool', yT[:, c, tsl], accy[:], tmpy[:], ALU.add, [accy, tmpy], [yT])
                S.fence()
                for c in range(KC):
                    cpy('pool' if c % 2 else 'dve', hT[:, c, :], yT[:, c, :], [yT], [hT])
            S.fence()
            inner.close()
            yTT = hT
            chk(6)

            if cast_done[0] < DEPTH and cast_done[0] == l + 1:
                cast_layer(cast_done[0])
                cast_done[0] += 1
            with ExitStack() as st:
                wo = sb(st, [128, KC, DM], BF16, "wo")
                wload(wo, wo[:, :, 0:512], ('out', l), woutb[l], 0, 512)
                wload(wo, wo[:, :, 512:1024], ('out', l), woutb[l], 512, 1024)
                xblk = [sb(st, [128, KC, 512], F32, "xblk") for _ in range(1)]
                sq = sb(st, [128, KC, 512], F32, "sq")
                rs = sb(st, [128, 512], F32, "rs")
                rs2 = sb(st, [128, 512], F32, "rs2")
                h2T = sb(st, [128, KC, 512], BF16, "h2T")
                uT = sb(st, [128, 32, 512], BF16, "uT")
                wu = [sb(st, [128, KC, 512], BF16, "wu") for _ in range(2)]
                wd_ = [sb(st, [128, 16, 256], BF16, "wd") for _ in range(2)]
                wdc = [0]
                rl = [sb(st, [128, 512], F32, "rl") for _ in range(2)]
                otm = [sb(st, [128, DM], F32, "otm") for _ in range(2)] if last_layer else None
                fin = sb(st, [128, KC, 512], F32, "fin") if last_layer else None
                pcn = [0]
                xsb2 = Buf('xs2')
                for tb in range(NB):
                    tsl = slice(tb * 512, (tb + 1) * 512)
                    xb_ = xblk[0]
                    dma(xb_[:], xs_d[s][:, :, tsl].rearrange("c p t -> p c t"), [xsb], [xb_])
                    for fc in range(KC):
                        pb = PS[pcn[0] % 2]
                        pcn[0] += 1
                        for kc in range(KC):
                            mm(pb[:], wo[:, kc, fc * 128:(fc + 1) * 128], yTT[:, kc, tsl], kc == 0, kc == KC - 1,
                               [wo, yTT], [pb])
                        tt('dve', xb_[:, fc, :], pb[:], xb_[:, fc, :], ALU.add, [pb, xb_], [xb_])
                    norm_block(st, xb_, 2 + l, lambda c: h2T[:, c, :], [h2T], 2, (sq, rs, rs2))
                    for fg in range(8):
                        wu_ = wu[fg % 2]
                        wload(wu_, wu_[:], ('up', l), wupb[l], fg * 512, (fg + 1) * 512)
                        for fc in range(4):
                            pb = PS[pcn[0] % 2]
                            r_ = rl[pcn[0] % 2]
                            pcn[0] += 1
                            for kc in range(KC):
                                mm(pb[:], wu_[:, kc, fc * 128:(fc + 1) * 128], h2T[:, kc, :], kc == 0, kc == KC - 1,
                                   [wu_, h2T], [pb])
                            act(r_[:], pb[:], AF.Relu, [pb], [r_])
                            tt('dve', uT[:, fg * 4 + fc, :], r_[:], r_[:], ALU.mult, [r_], [uT])
                    for f2 in range(4):
                        pbs = [PS[3 + (f2 % 2) * 2 + fc] for fc in range(2)]
                        for hk in range(2):
                            wdt = wd_[wdc[0] % 2]
                            wdc[0] += 1
                            dma(wdt[:], wdnb[l][hk * 2048:(hk + 1) * 2048, f2 * 256:(f2 + 1) * 256].rearrange(
                                "(kc p) f -> p kc f", p=128), wbuf[('dn', l)], [wdt])
                            for fc in range(2):
                                for kc in range(16):
                                    mm(pbs[fc][:], wdt[:, kc, fc * 128:(fc + 1) * 128], uT[:, hk * 16 + kc, :],
                                       hk == 0 and kc == 0, hk == 1 and kc == 15, [wdt, uT], [pbs[fc]])
                        for fc in range(2):
                            c = f2 * 2 + fc
                            tt('dve', xb_[:, c, :], pbs[fc][:], xb_[:, c, :], ALU.add, [pbs[fc], xb_], [xb_])
                    if dbg and s == 0 and l == 0:
                        dma(dbg_x[:, :, tsl].rearrange("c p t -> p c t"), xb_[:], [xb_], [])
                    if not last_layer:
                        dma(xs_d[s][:, :, tsl].rearrange("c p t -> p c t"), xb_[:], [xb_], [xsb2])
                    else:
                        if final:
                            norm_block(st, xb_, 4, lambda c: fin[:, c, :], [fin], 2, (sq, rs, rs2))
                        else:
                            for c in range(KC):
                                cpy('pool' if c % 2 else 'dve', fin[:, c, :], xb_[:, c, :], [xb_], [fin])
                        for t4 in range(4):
                            ot = otm[t4 % 2]
                            for half in range(2):
                                pb = PS[5 + half]
                                for c4 in range(4):
                                    c = half * 4 + c4
                                    tr(pb[:, c4 * 128:(c4 + 1) * 128], fin[:, c, t4 * 128:(t4 + 1) * 128], identF[:],
                                       [fin, identF], [pb])
                                evac(ot[:, half * 512:(half + 1) * 512], pb[:], [pb], [ot])
                            tt_ = tb * 4 + t4
                            dma(out_d[s][tt_ * 128:(tt_ + 1) * 128, :], ot[:], [ot], [])
                xsb = xsb2
            S.fence()
            lay.close()
        seqst.close()
        S.fence()

    try:
        body()
    except StopBuild:
        S.fence()
    S.emit(top)
    top.close()
    return nc, S


_CACHE = {}


def kernel(x, positions, g_mix, w_in, b_forget, w_branch, w_out, g_mlp, w_up, w_down, g_final):
    n = 8
    if 'nc' not in _CACHE:
        _CACHE['nc'] = build(2, 2)[0]
    nc = _CACHE['nc']
    f32 = lambda a: np.ascontiguousarray(np.asarray(a, dtype=np.float32))
    x = f32(x)
    pos = np.ascontiguousarray(np.asarray(positions, dtype=np.int32))
    shared = dict(g_mix=f32(g_mix), w_in=f32(w_in), b_forget=f32(b_forget), w_branch=f32(w_branch),
                  w_out=f32(w_out), g_mlp=f32(g_mlp), w_up=f32(w_up), w_down=f32(w_down), g_final=f32(g_final))
    in_maps = []
    for c in range(n):
        m = dict(shared)
        m['x'] = np.ascontiguousarray(x[2 * c:2 * c + 2])
        m['positions'] = np.ascontiguousarray(pos[2 * c:2 * c + 2])
        in_maps.append(m)
    res = run_bass_kernel_spmd(nc, in_maps, core_ids=list(range(n)))
    return np.concatenate([np.asarray(r["out"], dtype=np.float32) for r in res.results], axis=0)
```
